# Optimizing a Trainium2 kernel written in Bass

```python
import math
import jax, jax.numpy as jnp
from jax import lax
import numpy as np

D_MODEL = 1024
BATCH = 32
SEQ = 2048
DEPTH = 1

GRID_W = 64
CTX_LEN = 256
CONV_DIM = 512
CONV_WIDTH = 31
RET_HEADS = 4
RET_DK = 128
RET_DV = 256
RET_CHUNK = 128
ROPE_BASE = 10000.0
QK_DIM = RET_HEADS * RET_DK
V_DIM = RET_HEADS * RET_DV
N_EXPERTS = 16
EXPERT_HIDDEN = 1024
EC_FACTOR = 2
LN_EPS = 1e-5
DEEPNORM_ALPHA = (2.0 * DEPTH) ** 0.25
DEEPNORM_BETA = (8.0 * DEPTH) ** -0.25
U_END = 2 * CONV_DIM
Q_END = U_END + QK_DIM
K_END = Q_END + QK_DIM
V_END = K_END + V_DIM
G_END = V_END + V_DIM
GA_END = G_END + D_MODEL
IN_COLS = GA_END + D_MODEL

kernel_name = "hybrid_conv_retention_ec_moe_dit"


def _ln(x):
    xf = x.astype(jnp.float32)
    mu = jnp.mean(xf, axis=-1, keepdims=True)
    var = jnp.mean(jnp.square(xf - mu), axis=-1, keepdims=True)
    return ((xf - mu) * lax.rsqrt(var + LN_EPS)).astype(x.dtype)


def layer_norm(x, g, b):
    return _ln(x) * g + b


def modulate(x, shift, scale):
    return _ln(x) * (1.0 + scale) + shift


def to_heads(t, n_heads):
    b, L, _ = t.shape
    return t.reshape(b, L, n_heads, -1).transpose(0, 2, 1, 3)


def rope_2d(t, rows, cols):
    n_pairs_axis = RET_DK // 4
    freqs = ROPE_BASE ** (-jnp.arange(n_pairs_axis, dtype=jnp.float32) / n_pairs_axis)
    ang = jnp.concatenate([rows[:, None] * freqs, cols[:, None] * freqs], axis=-1)
    cos, sin = jnp.cos(ang).astype(t.dtype), jnp.sin(ang).astype(t.dtype)
    t1, t2 = t[..., 0::2], t[..., 1::2]
    return jnp.stack([t1 * cos - t2 * sin, t1 * sin + t2 * cos], axis=-1).reshape(t.shape)


def context_states(k, v, log_gamma_f, log_gamma_b):
    L = k.shape[2]
    t = jnp.arange(L, dtype=jnp.float32)
    w_f = jnp.exp(log_gamma_f.astype(jnp.float32)[:, None] * (L - 1 - t))
    w_b = jnp.exp(log_gamma_b.astype(jnp.float32)[:, None] * t)
    s_f = jnp.einsum('bhtd,ht,bhtv->bhdv', k, w_f, v).astype(jnp.float32)
    s_b = jnp.einsum('bhtd,ht,bhtv->bhdv', k, w_b, v).astype(jnp.float32)
    return s_f, s_b


def retention_chunkwise(q, k, v, log_gamma, s0, inclusive):
    b, h, L, _ = q.shape
    n = L // RET_CHUNK
    lg = log_gamma.astype(jnp.float32)
    pos = jnp.arange(RET_CHUNK, dtype=jnp.float32)
    diff = pos[:, None] - pos[None, :]
    mask = (diff >= 0) if inclusive else (diff > 0)
    intra_dec = jnp.where(mask[None], jnp.exp(lg[:, None, None] * jnp.maximum(diff, 0.0)[None]), 0.0)
    cross_dec = jnp.exp(lg[:, None] * (pos + 1.0))
    state_dec = jnp.exp(lg[:, None] * (RET_CHUNK - 1.0 - pos))
    chunk_dec = jnp.exp(lg * RET_CHUNK)

    def to_chunks(t):
        return t.reshape(b, h, n, RET_CHUNK, t.shape[-1]).transpose(2, 0, 1, 3, 4)

    def step(s, qkv):
        qc, kc, vc = qkv
        att = jnp.einsum('bhid,bhjd->bhij', qc, kc) * intra_dec
        o = (jnp.einsum('bhij,bhjv->bhiv', att, vc)
             + jnp.einsum('bhid,bhdv->bhiv', qc * cross_dec[..., None], s))
        s = s * chunk_dec[:, None, None] + jnp.einsum('bhjd,bhjv->bhdv', kc * state_dec[..., None], vc)
        return s, o

    _, o = lax.scan(step, s0.astype(jnp.float32), (to_chunks(q), to_chunks(k), to_chunks(v)))
    return o.transpose(1, 2, 0, 3, 4).reshape(b, h, L, -1)


def bidir_retention(q, k, v, lg_f, lg_b, s_f, s_b):
    o_f = retention_chunkwise(q, k, v, lg_f, s_f, True)
    flip = lambda t: jnp.flip(t, axis=2)
    o_b = flip(retention_chunkwise(flip(q), flip(k), flip(v), lg_b, s_b, False))
    return o_f + o_b


def head_group_norm(o, g, dtype):
    b, h, L, dv = o.shape
    of = o.astype(jnp.float32)
    mu = jnp.mean(of, axis=-1, keepdims=True)
    var = jnp.mean(jnp.square(of - mu), axis=-1, keepdims=True)
    on = (of - mu) * lax.rsqrt(var + LN_EPS)
    return on.transpose(0, 2, 1, 3).reshape(b, L, h * dv).astype(dtype) * g


def conformer_conv(u, conv_w, conv_b, ln_g, ln_b):
    a, gt = u[..., :CONV_DIM], u[..., CONV_DIM:]
    y = a * jax.nn.sigmoid(gt)
    y = lax.conv_general_dilated(y, conv_w[:, None, :], window_strides=(1,),
                                 padding=[(CONV_WIDTH // 2, CONV_WIDTH // 2)],
                                 dimension_numbers=('NWC', 'WIO', 'NWC'),
                                 feature_group_count=CONV_DIM) + conv_b
    return jax.nn.silu(layer_norm(y, ln_g, ln_b))


def token_mixer(z, rot, s_f, s_b, conv_w, conv_b, conv_ln_g, conv_ln_b, w_conv_out,
                lg_f, lg_b, ret_gn_g, w_ret_out, w_out):
    u = z[..., :U_END]
    q = to_heads(z[..., U_END:Q_END], RET_HEADS)
    k = to_heads(z[..., Q_END:K_END], RET_HEADS) * (RET_DK ** -0.5)
    v = to_heads(z[..., K_END:V_END], RET_HEADS)
    g_ret = z[..., V_END:G_END]
    g_a, g_b = z[..., G_END:GA_END], z[..., GA_END:IN_COLS]
    y_a = conformer_conv(u, conv_w, conv_b, conv_ln_g, conv_ln_b) @ w_conv_out
    if rot is not None:
        q, k = rope_2d(q, *rot), rope_2d(k, *rot)
    o = bidir_retention(q, k, v, lg_f, lg_b, s_f, s_b)
    y_b = (jax.nn.silu(g_ret) * head_group_norm(o, ret_gn_g, z.dtype)) @ w_ret_out
    y = jax.nn.sigmoid(g_a) * y_a + jax.nn.sigmoid(g_b) * y_b
    return y @ w_out


def expert_choice_ffn(h, w_router, w_gate, w_up, w_down):
    b, L, d = h.shape
    cap = EC_FACTOR * L // N_EXPERTS
    aff = jax.nn.softmax((h @ w_router).astype(jnp.float32), axis=-1)
    gates, idx = lax.top_k(aff.transpose(0, 2, 1), cap)
    xe = jax.vmap(lambda hb, ib: hb[ib])(h, idx)
    he = jax.nn.silu(jnp.einsum('becd,edf->becf', xe, w_gate)) * jnp.einsum('becd,edf->becf', xe, w_up)
    ye = jnp.einsum('becf,efd->becd', he, w_down) * gates[..., None].astype(h.dtype)
    return jax.vmap(lambda yb, ib: jnp.zeros((L, d), yb.dtype).at[ib.reshape(-1)].add(yb.reshape(-1, d)))(ye, idx)


def setup_inputs(seed: int = 0) -> dict:
    key = jax.random.key(seed)
    ks = jax.random.split(key, 25)
    f32 = jnp.float32
    nrm = lambda k, shape, s: jax.random.normal(k, shape, f32) * s
    base_decay = jnp.log(1.0 - 2.0 ** (-5.0 - jnp.arange(RET_HEADS, dtype=f32)))
    return {
        "x": nrm(ks[0], (BATCH, SEQ, D_MODEL), 1.0),
        "c": nrm(ks[1], (BATCH, D_MODEL), 1.0),
        "ctx": nrm(ks[2], (BATCH, CTX_LEN, D_MODEL), 1.0),
        "c_ctx": nrm(ks[3], (D_MODEL,), 1.0),
        "w_ada": nrm(ks[4], (DEPTH, D_MODEL, 6 * D_MODEL), 0.5 * D_MODEL ** -0.5),
        "b_ada": nrm(ks[5], (DEPTH, 6 * D_MODEL), 0.02),
        "w_in": nrm(ks[6], (DEPTH, D_MODEL, IN_COLS), D_MODEL ** -0.5),
        "conv_w": nrm(ks[7], (DEPTH, CONV_WIDTH, CONV_DIM), CONV_WIDTH ** -0.5),
        "conv_b": nrm(ks[8], (DEPTH, CONV_DIM), 0.02),
        "conv_ln_g": 1.0 + nrm(ks[9], (DEPTH, CONV_DIM), 0.02),
        "conv_ln_b": nrm(ks[10], (DEPTH, CONV_DIM), 0.02),
        "w_conv_out": nrm(ks[11], (DEPTH, CONV_DIM, D_MODEL), DEEPNORM_BETA * CONV_DIM ** -0.5),
        "log_decay_f": base_decay[None] * jnp.exp(nrm(ks[12], (DEPTH, RET_HEADS), 0.1)),
        "log_decay_b": base_decay[None] * jnp.exp(nrm(ks[13], (DEPTH, RET_HEADS), 0.1)),
        "ret_gn_g": 1.0 + nrm(ks[14], (DEPTH, V_DIM), 0.02),
        "w_ret_out": nrm(ks[15], (DEPTH, V_DIM, D_MODEL), DEEPNORM_BETA * V_DIM ** -0.5),
        "w_out": nrm(ks[16], (DEPTH, D_MODEL, D_MODEL), DEEPNORM_BETA * D_MODEL ** -0.5),
        "ln1_g": 1.0 + nrm(ks[17], (DEPTH, D_MODEL), 0.02),
        "ln1_b": nrm(ks[18], (DEPTH, D_MODEL), 0.02),
        "w_router": nrm(ks[19], (DEPTH, D_MODEL, N_EXPERTS), D_MODEL ** -0.5),
        "w_gate": nrm(ks[20], (DEPTH, N_EXPERTS, D_MODEL, EXPERT_HIDDEN), D_MODEL ** -0.5),
        "w_up": nrm(ks[21], (DEPTH, N_EXPERTS, D_MODEL, EXPERT_HIDDEN), D_MODEL ** -0.5),
        "w_down": nrm(ks[22], (DEPTH, N_EXPERTS, EXPERT_HIDDEN, D_MODEL), DEEPNORM_BETA * EXPERT_HIDDEN ** -0.5),
        "ln2_g": 1.0 + nrm(ks[23], (DEPTH, D_MODEL), 0.02),
        "ln2_b": nrm(ks[24], (DEPTH, D_MODEL), 0.02),
    }


def reference(x, c, ctx, c_ctx, w_ada, b_ada, w_in, conv_w, conv_b, conv_ln_g, conv_ln_b, w_conv_out,
              log_decay_f, log_decay_b, ret_gn_g, w_ret_out, w_out, ln1_g, ln1_b,
              w_router, w_gate, w_up, w_down, ln2_g, ln2_b):
    L = x.shape[1]
    n_rows = L // GRID_W
    rows = jnp.repeat(jnp.arange(n_rows, dtype=jnp.float32), GRID_W)
    cols = jnp.tile(jnp.arange(GRID_W, dtype=jnp.float32), n_rows)
    for l in range(DEPTH):
        last = l == DEPTH - 1
        mod = jax.nn.silu(c) @ w_ada[l] + b_ada[l]
        mod_c = jax.nn.silu(c_ctx) @ w_ada[l] + b_ada[l]
        sh1, sc1, g1, sh2, sc2, g2 = jnp.split(mod[:, None, :], 6, axis=-1)
        csh1, csc1, cg1, csh2, csc2, cg2 = jnp.split(mod_c, 6, axis=-1)
        mix_w = (conv_w[l], conv_b[l], conv_ln_g[l], conv_ln_b[l], w_conv_out[l],
                 log_decay_f[l], log_decay_b[l], ret_gn_g[l], w_ret_out[l], w_out[l])
        h = modulate(x, sh1, sc1)
        h_c = modulate(ctx, csh1, csc1)
        if last:
            kv_c = h_c @ w_in[l][:, Q_END:V_END]
            k_c, v_c = kv_c[..., :QK_DIM], kv_c[..., QK_DIM:]
        else:
            z_c = h_c @ w_in[l]
            k_c, v_c = z_c[..., Q_END:K_END], z_c[..., K_END:V_END]
        s_f, s_b = context_states(to_heads(k_c, RET_HEADS) * (RET_DK ** -0.5), to_heads(v_c, RET_HEADS),
                                  log_decay_f[l], log_decay_b[l])
        y = token_mixer(h @ w_in[l], (rows, cols), s_f, s_b, *mix_w)
        x = layer_norm(DEEPNORM_ALPHA * x + g1 * y, ln1_g[l], ln1_b[l])
        if not last:
            zero_state = jnp.zeros_like(s_f)
            y_c = token_mixer(z_c, None, zero_state, zero_state, *mix_w)
            ctx = layer_norm(DEEPNORM_ALPHA * ctx + cg1 * y_c, ln1_g[l], ln1_b[l])
        h = modulate(x, sh2, sc2)
        x = layer_norm(DEEPNORM_ALPHA * x + g2 * expert_choice_ffn(h, w_router[l], w_gate[l], w_up[l], w_down[l]),
                       ln2_g[l], ln2_b[l])
        if not last:
            h_c = modulate(ctx, csh2, csc2)
            ctx = layer_norm(DEEPNORM_ALPHA * ctx + cg2 * expert_choice_ffn(h_c, w_router[l], w_gate[l], w_up[l], w_down[l]),
                             ln2_g[l], ln2_b[l])
    return x
```

```python
import contextlib
import math
import os
import numpy as np
import concourse.bass as bass
import concourse.mybir as mybir
from concourse.bass_utils import run_bass_kernel_spmd

dt = mybir.dt
F32, BF16, I32, U32 = dt.float32, dt.bfloat16, dt.int32, dt.uint32
AF = mybir.ActivationFunctionType
ALU = mybir.AluOpType

NS = 4
L = 2048
D = 1024
NCH = 16
ALPHA = 2.0 ** 0.25
EPS = 1e-5
DKS = 128.0 ** -0.5
NE = 16
CAP = 256


class V:
    def __init__(self, t, ap):
        self.t, self.ap = t, ap

    def __getitem__(self, k):
        return V(self.t, self.ap[k])

    def re(self, s, **kw):
        return V(self.t, self.ap.rearrange(s, **kw))

    def bc(self, shape):
        return V(self.t, self.ap.to_broadcast(shape))

    def un(self, ax):
        return V(self.t, self.ap.unsqueeze(ax))

    def bitcast(self, d):
        return V(self.t, self.ap.bitcast(d))


class T:
    def __init__(self, ap):
        self.ap = ap
        self.w = None
        self.r = {}

    def __getitem__(self, k):
        return V(self, self.ap[k])

    def v(self):
        return V(self, self.ap)


class Rot:
    def __init__(self, ts):
        self.ts = ts
        self.i = 0

    def next(self):
        t = self.ts[self.i % len(self.ts)]
        self.i += 1
        t.gen = getattr(t, "gen", 0) + 1
        return t


class _Stop(Exception):
    pass


def _chk(name):
    if os.environ.get("KSTOP") == name:
        raise _Stop()


class Sched:
    N_DMA_SEMS = 40

    def __init__(self, nc, es):
        self.nc = nc
        self.eng = {"pe": nc.tensor, "act": nc.scalar, "dve": nc.vector, "pool": nc.gpsimd, "sp": nc.sync}
        self.semobj = {}
        self.cnt = {}
        for k in self.eng:
            self.semobj[k] = es.enter_context(nc.semaphore("s_" + k))
            self.cnt[k] = 0
        self.seen = {k: {} for k in self.eng}
        self.dma_keys = []
        for i in range(self.N_DMA_SEMS):
            key = "d%d" % i
            self.semobj[key] = es.enter_context(nc.semaphore("s_" + key))
            self.cnt[key] = 0
            self.dma_keys.append(key)
        self.dma_rr = 0
        self.n_ins = 0

    def _collect(self, ek, reads, writes):
        waits = {}

        def need(dep):
            if dep is None:
                return
            key, val = dep
            if waits.get(key, 0) < val:
                waits[key] = val

        for t in reads:
            need(t.w)
        for t in writes:
            if t.w is not None and t.w[0] != ek:
                need(t.w)
            for k, v in t.r.items():
                if k != ek:
                    need((k, v))
        return waits

    def _emit_waits(self, ek, waits):
        E = self.eng[ek]
        for key, val in waits.items():
            if self.seen[ek].get(key, 0) >= val:
                continue
            E.wait_ge(self.semobj[key], val)
            self.seen[ek][key] = val
            self.n_ins += 1

    def op(self, ek, fn, reads=(), writes=(), inc=True):
        waits = self._collect(ek, reads, writes)
        self._emit_waits(ek, waits)
        ins = fn(self.eng[ek])
        self.n_ins += 1
        if inc:
            self.cnt[ek] += 1
            ins.then_inc(self.semobj[ek], 1)
            c = self.cnt[ek]
        else:
            c = self.cnt[ek] + 1
        for t in reads:
            t.r[ek] = c
        for t in writes:
            t.w = (ek, c)
            t.r = {}
        return ins

    def dma(self, q, fn, reads=(), writes=()):
        key = self.dma_keys[self.dma_rr]
        self.dma_rr = (self.dma_rr + 1) % len(self.dma_keys)
        waits = self._collect(key, reads, writes)
        if self.cnt[key] > 0 and waits.get(key, 0) < self.cnt[key]:
            waits[key] = self.cnt[key]
        self._emit_waits(q, waits)
        ins = fn(self.eng[q])
        self.n_ins += 1
        self.cnt[key] += 16
        ins.then_inc(self.semobj[key], 16)
        c = self.cnt[key]
        for t in reads:
            t.r[key] = c
        for t in writes:
            t.w = (key, c)
            t.r = {}
        return ins

    def barrier(self, engines=("pe", "act", "dve", "pool", "sp")):
        for ek in engines:
            waits = {}
            for k, c in self.cnt.items():
                if k != ek and c > 0:
                    waits[k] = c
            self._emit_waits(ek, waits)


def build_program(debug=False):
    holder = {}
    try:
        return _build_inner(debug, holder)
    except _Stop:
        return holder["nc"]


def _build_inner(debug, holder):
    nc = bass.Bass("TRN2", target_bir_lowering=False)
    holder["nc"] = nc

    def din(name, shape, dtype=F32):
        return T(nc.dram_tensor(name, list(shape), dtype, kind="ExternalInput").ap())

    x_d = din("x", [NS, L, D])
    ctx_d = din("ctx", [NS, 256, D])
    cT_d = din("cT", [128, 8, 5])
    w_ada_d = din("w_ada", [D, 6 * D])
    b_ada_d = din("b_ada", [1, 6 * D])
    w_in_d = din("w_in", [D, 6 * D])
    convw_d = din("convw", [32, 512])
    cln_g_d = din("conv_ln_g", [1, 512])
    cln_b_d = din("conv_ln_b", [1, 512])
    w_co_d = din("w_conv_out", [512, D])
    lgf_d = din("log_decay_f", [1, 4])
    lgb_d = din("log_decay_b", [1, 4])
    gn_g_d = din("ret_gn_g", [1, D])
    w_ro_d = din("w_ret_out", [D, D])
    w_out_d = din("w_out", [D, D])
    ln1_g_d = din("ln1_g", [1, D])
    ln1_b_d = din("ln1_b", [1, D])
    w_r_d = din("w_router", [D, NE])
    w_gate_d = din("w_gate", [NE, D, D])
    w_up_d = din("w_up", [NE, D, D])
    w_down_d = din("w_down", [NE, D, D])
    ln2_g_d = din("ln2_g", [1, D])
    ln2_b_d = din("ln2_b", [1, D])
    out_d = T(nc.dram_tensor("out", [NS * L, D], F32, kind="ExternalOutput").ap())

    dk = dict(kind="ExternalOutput") if debug else {}
    MOD = T(nc.dram_tensor("mod_s", [5, 6 * D], F32, **dk).ap())
    WIN = T(nc.dram_tensor("win_s", [128, 8, 6 * D], BF16).ap())
    WRO = T(nc.dram_tensor("wro_s", [128, 8, D], BF16).ap())
    WOUT = T(nc.dram_tensor("wout_s", [128, 8, D], BF16).ap())
    WCO = T(nc.dram_tensor("wco_s", [128, 4, D], BF16).ap())
    H2 = T(nc.dram_tensor("h2_s", [NS * L, D], BF16, **dk).ap())
    ACCALL = nc.dram_tensor("acc_s", [NS * L, D], F32, **dk).ap()
    ACC = [T(ACCALL[b * L:(b + 1) * L, :]) for b in range(NS)]
    if debug:
        DBG_IDX = T(nc.dram_tensor("dbg_idx", [128, 2, NS * NE], I32, kind="ExternalOutput").ap())
        DBG_GT = T(nc.dram_tensor("dbg_gt", [128, 2, NS * NE], F32, kind="ExternalOutput").ap())
        DBG_AFF = T(nc.dram_tensor("dbg_aff", [128, NCH, NS * NE], F32, kind="ExternalOutput").ap())
        DBG_X1 = T(nc.dram_tensor("dbg_x1", [NS * L, D], F32, kind="ExternalOutput").ap())
    dbg = {}

    es = contextlib.ExitStack()
    with es:
        S = Sched(nc, es)

        uniq = [0]

        def sbt(sc, name, shape, dtype):
            uniq[0] += 1
            return T(sc.enter_context(nc.sbuf_tensor("%s_u%d" % (name, uniq[0]), list(shape), dtype))[:])

        def rot(sc, name, shape, dtype, n):
            return Rot([sbt(sc, "%s%d" % (name, i), shape, dtype) for i in range(n)])

        def rd(*vs):
            return [v.t for v in vs if isinstance(v, V)]

        def apof(x):
            return x.ap if isinstance(x, V) else x

        def ACT(out, in_, func, bias=None, scale=None, accum=None):
            kw = {}
            if bias is not None:
                kw["bias"] = apof(bias)
            if scale is not None:
                kw["scale"] = apof(scale)
            if accum is not None:
                kw["accum_out"] = accum.ap
            S.op("act", lambda e: e.activation(out=out.ap, in_=in_.ap, func=func, **kw),
                 reads=rd(in_, bias, scale), writes=[out.t] + ([accum.t] if accum is not None else []))

        def TT(ek, out, a, b, op):
            S.op(ek, lambda e: e.tensor_tensor(out=out.ap, in0=a.ap, in1=b.ap, op=op), reads=rd(a, b), writes=[out.t])

        def TS(ek, out, a, s1, op0, s2=None, op1=None):
            kw = {}
            if op1 is not None:
                kw["op1"] = op1
            S.op(ek, lambda e: e.tensor_scalar(out=out.ap, in0=a.ap, scalar1=apof(s1), scalar2=apof(s2), op0=op0, **kw),
                 reads=rd(a, s1, s2), writes=[out.t])

        def STT(out, a, s, b, op0, op1):
            S.op("dve", lambda e: e.scalar_tensor_tensor(out=out.ap, in0=a.ap, scalar=apof(s), in1=b.ap, op0=op0, op1=op1),
                 reads=rd(a, s, b), writes=[out.t])

        def CP(ek, out, in_):
            if ek == "act":
                ACT(out, in_, AF.Copy)
            else:
                S.op(ek, lambda e: e.tensor_copy(out=out.ap, in_=in_.ap), reads=rd(in_), writes=[out.t])

        def MSET(ek, out, val):
            S.op(ek, lambda e: e.memset(out.ap, val), writes=[out.t])

        def MM(out, lhsT, rhs, start, stop, inc):
            S.op("pe", lambda e: e.matmul(out.ap, lhsT=lhsT.ap, rhs=rhs.ap, start=start, stop=stop),
                 reads=rd(lhsT, rhs), writes=[out.t], inc=inc)

        def TR(out, in_, ident, inc):
            S.op("pe", lambda e: e.transpose(out=out.ap, in_=in_.ap, identity=ident.ap), reads=rd(in_, ident), writes=[out.t], inc=inc)

        def DMA(out, in_, q="sp", **kw):
            S.dma(q, lambda e: e.dma_start(out=out.ap, in_=in_.ap, **kw), reads=[in_.t], writes=[out.t])

        P = es
        PA = Rot([T(P.enter_context(nc.psum_tensor("pa%d" % i, [128, 1024], F32))[:]) for i in range(3)])
        PTf = Rot([T(P.enter_context(nc.psum_tensor("pt%d" % i, [128, 512], F32))[:]) for i in range(2)])

        def pt_next():
            return PTf.next().v().bitcast(BF16)

        identb = sbt(P, "identb", [128, 128], BF16)
        identf = sbt(P, "identf", [128, 128], F32)
        nhalf = sbt(P, "nhalf", [128, 4], F32)
        COS = sbt(P, "COS", [128, 16, 64], F32)
        SIN = sbt(P, "SIN", [128, 16, 64], F32)
        lgf = sbt(P, "lgf", [128, 4], F32)
        lgb = sbt(P, "lgb", [128, 4], F32)
        DF = sbt(P, "DF", [128, 4], F32)
        DB = sbt(P, "DB", [128, 4], F32)
        CF = sbt(P, "CF", [128, 4], F32)
        CB = sbt(P, "CB", [128, 4], F32)
        G128F = sbt(P, "G128F", [128, 4], F32)
        G128B = sbt(P, "G128B", [128, 4], F32)
        MASKT = sbt(P, "MASKT", [128, 4, 128], F32)
        cwT = sbt(P, "cwT", [128, 4, 32], F32)
        clnT = sbt(P, "clnT", [128, 4, 2], F32)
        AFF = sbt(P, "AFF", [128, NCH, NS * NE], F32)
        WR = sbt(P, "WR", [128, 8, NE], BF16)
        st_rot = rot(P, "lnst", [128, 4, 6], F32, 8)
        mv_rot = rot(P, "lnmv", [128, 4, 2], F32, 8)
        rs_rot = rot(P, "lnrs", [128, 8], F32, 8)

        MSET("pool", nhalf.v(), -0.5)
        MSET("pool", identb.v(), 1.0)
        S.op("pool", lambda e: e.affine_select(out=identb.ap, in_=identb.ap, pattern=[[-1, 128]], compare_op=ALU.is_equal,
                                               fill=0.0, base=0, channel_multiplier=1), reads=[identb], writes=[identb])
        MSET("pool", identf.v(), 1.0)
        S.op("pool", lambda e: e.affine_select(out=identf.ap, in_=identf.ap, pattern=[[-1, 128]], compare_op=ALU.is_equal,
                                               fill=0.0, base=0, channel_multiplier=1), reads=[identf], writes=[identf])

        def ln_stats(src, F, eps=EPS):
            nch = max(1, F // 512)
            w = F // nch
            st = st_rot.next()
            for i in range(nch):
                S.op("dve", lambda e, i=i: e.bn_stats(out=st.ap[:, i, :], in_=src.ap[:, i * w:(i + 1) * w]), reads=[src.t], writes=[st])
            mv = mv_rot.next()
            S.op("dve", lambda e: e.bn_aggr(out=mv.ap[:, 0, :], in_=st.ap[:, 0:nch, :].rearrange("p a b -> p (a b)")), reads=[st], writes=[mv])
            rs = rs_rot.next()
            TS("dve", rs[:, 0:1], mv[:, 0, 1:2], eps, ALU.add)
            TT("pool", rs[:, 0:1], rs[:, 0:1], nhalf[:, 0:1], ALU.pow)
            return rs, mv

        def ln_apply(out, in_, st):
            rs, mv = st
            ACT(rs[:, 2:3], mv[:, 0, 0:1], AF.Identity, scale=rs[:, 0:1])
            ACT(rs[:, 1:2], rs[:, 2:3], AF.Identity, scale=-1.0)
            ACT(out, in_, AF.Identity, bias=rs[:, 1:2], scale=rs[:, 0:1])

        def transposeN(dst, src, n, evac="act"):
            pt = pt_next()
            for c in range(n):
                TR(pt[:, c * 128:(c + 1) * 128], src[:, c * 128:(c + 1) * 128], identb.v(), inc=(c == n - 1))
            CP(evac, dst, pt[:, 0:n * 128].re("p (c t) -> p c t", c=n))

        def pipeline(N, stages):
            cs = [dict() for _ in range(N)]
            ent = [(e if isinstance(e, tuple) else (i, e)) for i, e in enumerate(stages)]
            maxlag = max(l for l, _ in ent)
            if os.environ.get("KCHECK"):
                def tiles_of(v):
                    if isinstance(v, T):
                        yield v
                    elif isinstance(v, V):
                        yield v.t
                    elif isinstance(v, (tuple, list)):
                        for x in v:
                            yield from tiles_of(x)

                class CD(dict):
                    def __setitem__(self, k, v):
                        dict.__setitem__(self, k, v)
                        self.__dict__.setdefault("g", {})[k] = [(t, getattr(t, "gen", 0)) for t in tiles_of(v)]

                    def __getitem__(self, k):
                        for t, g in self.__dict__.get("g", {}).get(k, []):
                            assert getattr(t, "gen", 0) == g, "stale rotating buffer %r" % (k,)
                        return dict.__getitem__(self, k)

                cs = [CD() for _ in range(N)]
            for it in range(N + maxlag):
                for lag, fn in ent:
                    n = it - lag
                    if 0 <= n < N:
                        fn(n, cs[n])

        with contextlib.ExitStack() as sc:
            stg = rot(sc, "stg", [128, 8, 512], F32, 5)
            stb = rot(sc, "stb", [128, 8, 512], BF16, 3)
            jobs = []
            win_v = w_in_d.v().re("(c p) f -> p c f", p=128)
            for g in range(12):
                jobs.append((win_v[:, :, g * 512:(g + 1) * 512], WIN[:, :, g * 512:(g + 1) * 512], 8))
            for (src_d, dst) in ((w_ro_d, WRO), (w_out_d, WOUT)):
                sv = src_d.v().re("(c p) f -> p c f", p=128)
                for g in range(2):
                    jobs.append((sv[:, :, g * 512:(g + 1) * 512], dst[:, :, g * 512:(g + 1) * 512], 8))
            sv = w_co_d.v().re("(c p) f -> p c f", p=128)
            for g in range(2):
                jobs.append((sv[:, :, g * 512:(g + 1) * 512], WCO[:, :, g * 512:(g + 1) * 512], 4))
            wr32 = sbt(sc, "wr32", [128, 8, NE], F32)
            DMA(wr32.v(), w_r_d.v().re("(c p) e -> p c e", p=128))
            CP("dve", WR.v(), wr32.v())
            scT = sbt(sc, "scT", [128, 8, 5], F32)
            DMA(scT.v(), cT_d.v())
            ACT(scT.v(), scT.v(), AF.Silu)
            bada = sbt(sc, "bada", [5, 6 * D], F32)
            DMA(bada.v(), V(b_ada_d, b_ada_d.ap.partition_broadcast(5)))
            modg = rot(sc, "modg", [5, 512], F32, 3)
            g8 = sbt(sc, "g8", [8, 128], F32)
            DMA(g8.v(), gn_g_d.v().re("o (c p) -> (o c) p", p=128))
            gT = sbt(sc, "gT", [128, 8], F32)
            pa = PA.next()
            TR(pa[:, 0:8], g8.v(), identf[0:8, 0:8], inc=True)
            CP("dve", gT.v(), pa[:, 0:8])
            items = [("ada", g) for g in range(12)] + [("cast", j) for j in jobs]

            def su_load(i, c):
                kind, arg = items[i]
                a = stg.next()
                if kind == "ada":
                    DMA(a.v(), w_ada_d.v().re("(c p) f -> p c f", p=128)[:, :, arg * 512:(arg + 1) * 512])
                else:
                    DMA(a[:, 0:arg[2], :], arg[0])
                c["a"] = a

            def su_comp(i, c):
                kind, arg = items[i]
                a = c["a"]
                if kind == "ada":
                    g = arg
                    pa = PA.next()
                    for kc in range(8):
                        MM(pa[0:5, 0:512], scT[:, kc, :], a[:, kc, :], start=(kc == 0), stop=(kc == 7), inc=(kc == 7))
                    mg = modg.next()
                    if g in (2, 3, 8, 9):
                        STT(mg.v(), pa[0:5, 0:512], 1.0, bada[:, g * 512:(g + 1) * 512], ALU.add, ALU.add)
                    else:
                        TT("dve", mg.v(), pa[0:5, 0:512], bada[:, g * 512:(g + 1) * 512], ALU.add)
                    c["o"] = mg
                else:
                    nc_ = arg[2]
                    b_ = stb.next()
                    if arg[1].t is WRO:
                        TT("dve", a[:, 0:nc_, :], a[:, 0:nc_, :], gT.v().un(2).bc([128, 8, 512]), ALU.mult)
                    CP("dve" if i % 2 == 0 else "act", b_[:, 0:nc_, :], a[:, 0:nc_, :])
                    c["o"] = b_

            def su_store(i, c):
                kind, arg = items[i]
                if kind == "ada":
                    DMA(MOD[:, arg * 512:(arg + 1) * 512], c["o"].v())
                else:
                    DMA(arg[1], c["o"][:, 0:arg[2], :])

            pipeline(len(items), [(0, su_load), (3, su_comp), (4, su_store)])
            pcol_i = sbt(sc, "pcol_i", [128, 1], I32)
            pcol = sbt(sc, "pcol", [128, 1], F32)
            S.op("pool", lambda e: e.iota(pcol_i.ap, pattern=[[0, 1]], base=0, channel_multiplier=1), writes=[pcol_i])
            CP("dve", pcol.v(), pcol_i.v())
            hi = sbt(sc, "hi", [128, 1], F32)
            TS("dve", hi.v(), pcol.v(), 64.0, ALU.is_ge)
            colp = sbt(sc, "colp", [128, 1], F32)
            STT(colp.v(), hi.v(), -64.0, pcol.v(), ALU.mult, ALU.add)
            rown_i = sbt(sc, "rown_i", [128, 16], I32)
            rown = sbt(sc, "rown", [128, 16], F32)
            S.op("pool", lambda e: e.iota(rown_i.ap, pattern=[[2, 16]], base=0, channel_multiplier=0), writes=[rown_i])
            CP("dve", rown.v(), rown_i.v())
            TS("dve", rown.v(), rown.v(), hi.v(), ALU.add)
            fr_i = sbt(sc, "fr_i", [128, 32], I32)
            fr = sbt(sc, "fr", [128, 32], F32)
            S.op("pool", lambda e: e.iota(fr_i.ap, pattern=[[1, 32]], base=0, channel_multiplier=0), writes=[fr_i])
            CP("dve", fr.v(), fr_i.v())
            ACT(fr.v(), fr.v(), AF.Exp, scale=-math.log(10000.0) / 32.0)
            ang = sbt(sc, "ang", [128, 16, 64], F32)
            for n in range(16):
                TS("dve", ang[:, n, 0:32], fr.v(), rown[:, n:n + 1], ALU.mult)
                TS("dve", ang[:, n, 32:64], fr.v(), colp.v(), ALU.mult)
            yq = sbt(sc, "yq", [128, 1024], F32)
            yi = sbt(sc, "yi", [128, 1024], I32)
            yf = sbt(sc, "yf", [128, 1024], F32)
            ngm = sbt(sc, "ngm", [128, 1024], F32)
            angf = ang.v().re("p n i -> p (n i)")
            for (dst, shift) in ((SIN, 0.0), (COS, math.pi / 2.0)):
                TS("dve", yq.v(), angf, 1.0 / (2 * math.pi), ALU.mult, 0.5 + shift / (2 * math.pi), ALU.add)
                CP("dve", yi.v(), yq.v())
                CP("dve", yf.v(), yi.v())
                TT("dve", yq.v(), yq.v(), yf.v(), ALU.subtract)
                TS("dve", ngm.v(), yq.v(), 0.0, ALU.is_lt)
                TT("dve", yq.v(), yq.v(), ngm.v(), ALU.add)
                TS("dve", yq.v(), yq.v(), -0.5, ALU.add, 2 * math.pi, ALU.mult)
                TS("dve", yq.v(), yq.v(), -math.pi, ALU.max, math.pi, ALU.min)
                ACT(dst.v().re("p n i -> p (n i)"), yq.v(), AF.Sin)
            DMA(lgf.v(), V(lgf_d, lgf_d.ap.partition_broadcast(128)))
            DMA(lgb.v(), V(lgb_d, lgb_d.ap.partition_broadcast(128)))
            tmpc = sbt(sc, "tmpc", [128, 1], F32)
            tmp4 = sbt(sc, "tmp4", [128, 4], F32)

            def dec_table(dst, lg, mul, add, post):
                TS("dve", tmpc.v(), pcol.v(), float(mul), ALU.mult, float(add), ALU.add)
                TS("dve", tmp4.v(), lg.v(), tmpc.v(), ALU.mult)
                ACT(tmp4.v(), tmp4.v(), AF.Exp)
                TS("dve", dst.v(), tmp4.v(), float(post), ALU.mult)

            dec_table(DF, lgf, -1.0, 127.0, DKS)
            dec_table(DB, lgb, 1.0, 0.0, DKS)
            dec_table(CF, lgf, 1.0, 1.0, 1.0)
            dec_table(CB, lgb, -1.0, 128.0, 1.0)
            dec_table(G128F, lgf, 0.0, 128.0, 1.0)
            dec_table(G128B, lgb, 0.0, 128.0, 1.0)
            dif_i = sbt(sc, "dif_i", [128, 128], I32)
            dpos = sbt(sc, "dpos", [128, 128], F32)
            dneg = sbt(sc, "dneg", [128, 128], F32)
            S.op("pool", lambda e: e.iota(dif_i.ap, pattern=[[1, 128]], base=0, channel_multiplier=-1), writes=[dif_i])
            CP("dve", dpos.v(), dif_i.v())
            TS("dve", dneg.v(), dpos.v(), -1.0, ALU.mult, 0.0, ALU.max)
            TS("dve", dpos.v(), dpos.v(), 0.0, ALU.max)
            mtmp = sbt(sc, "mtmp", [128, 128], F32)
            for h in range(4):
                TS("dve", mtmp.v(), dpos.v(), lgf[:, h:h + 1], ALU.mult)
                STT(mtmp.v(), dneg.v(), lgb[:, h:h + 1], mtmp.v(), ALU.mult, ALU.add)
                ACT(mtmp.v(), mtmp.v(), AF.Exp)
                TS("dve", MASKT[:, h, :], mtmp.v(), DKS, ALU.mult)
            cw = sbt(sc, "cw", [32, 512], F32)
            DMA(cw.v(), convw_d.v())
            pa = PA.next()
            for cc in range(4):
                TR(pa[:, cc * 32:(cc + 1) * 32], cw[:, cc * 128:(cc + 1) * 128], identf[0:32, 0:32], inc=(cc == 3))
            CP("dve", cwT.v(), pa[:, 0:128].re("p (c k) -> p c k", c=4))
            cl2 = sbt(sc, "cl2", [2, 512], F32)
            DMA(cl2[0:1, :], cln_g_d.v())
            DMA(cl2[1:2, :], cln_b_d.v())
            pa = PA.next()
            for cc in range(4):
                TR(pa[:, cc * 2:(cc + 1) * 2], cl2[:, cc * 128:(cc + 1) * 128], identf[0:2, 0:2], inc=(cc == 3))
            CP("dve", clnT.v(), pa[:, 0:8].re("p (c k) -> p c k", c=4))
            S.barrier()

        def mm_wide(ps, lhs_fn, w, nk):
            for hf in range(2):
                for kc in range(nk):
                    MM(ps[:, hf * 512:(hf + 1) * 512], lhs_fn(kc), w[:, kc, hf * 512:(hf + 1) * 512],
                       start=(kc == 0), stop=(kc == nk - 1), inc=(kc == nk - 1))

        for b in range(NS if "KSTOP" not in os.environ else 1):
            with contextlib.ExitStack() as sA:
                hT_all = sbt(sA, "hT", [128, 8, 18 * 128], BF16)
                hT = [T(hT_all.ap[:, :, j * 128:(j + 1) * 128]) for j in range(18)]
                VS_all = sbt(sA, "VS", [128, NCH, 1024], BF16)
                VS = [T(VS_all.ap[:, n, :]) for n in range(NCH)]

                def load_bc(sc, name, src_t, row, c0, n=1024):
                    t = sbt(sc, name, [128, n], F32)
                    DMA(t.v(), V(src_t, src_t.ap[row:row + 1, c0:c0 + n].partition_broadcast(128)))
                    return t

                with contextlib.ExitStack() as sc:
                    SH1 = load_bc(sc, "SH1", MOD, b, 0)
                    SC1 = load_bc(sc, "SC1", MOD, b, 1024)
                    SH1c = load_bc(sc, "SH1c", MOD, 4, 0)
                    SC1c = load_bc(sc, "SC1c", MOD, 4, 1024)
                    xin = rot(sc, "xin", [128, 1024], F32, 5)
                    xn = rot(sc, "xn", [128, 1024], F32, 2)
                    hb = rot(sc, "hb", [128, 1024], BF16, 3)

                    def s0_l(j, c):
                        xi = xin.next()
                        if j < 2:
                            DMA(xi.v(), ctx_d[b, j * 128:(j + 1) * 128, :])
                        else:
                            DMA(xi.v(), x_d[b, (j - 2) * 128:(j - 1) * 128, :])
                        c["xi"] = xi

                    def s0_l2(j, c):
                        pass

                    def s0_a(j, c):
                        c["rs"] = ln_stats(c["xi"].v(), 1024)

                    def s0_b(j, c):
                        sc_t, sh_t = (SC1c, SH1c) if j < 2 else (SC1, SH1)
                        xnt = xn.next()
                        ln_apply(xnt.v(), c["xi"].v(), c["rs"])
                        TT("dve", xnt.v(), xnt.v(), sc_t.v(), ALU.mult)
                        hbt = hb.next()
                        TT("pool", hbt.v(), xnt.v(), sh_t.v(), ALU.add)
                        c["hb"] = hbt

                    def s0_c(j, c):
                        transposeN(hT[j].v(), c["hb"].v(), 8)

                    pipeline(18, [s0_l, s0_l2, s0_a, s0_b, s0_c])
                    S.barrier()
                    _chk("s0")

                with contextlib.ExitStack() as sR:
                    kT_all = sbt(sR, "kT", [128, 4, L], BF16)
                    kT = [T(kT_all.ap[:, :, n * 128:(n + 1) * 128]) for n in range(NCH)]
                    KR_all = sbt(sR, "KR", [128, NCH, 512], BF16)
                    KR = [T(KR_all.ap[:, n, :].rearrange("p (h d) -> p h d", h=4)) for n in range(NCH)]
                    SFrun = sbt(sR, "SFrun", [128, 1024], F32)
                    SB_all = sbt(sR, "SB", [128, NCH, 1024], BF16)
                    SB = [T(SB_all.ap[:, n, :]) for n in range(NCH)]

                    def rope(ps, dst, n, tmp_rot):
                        pv = ps.re("p (h i two) -> p h i two", h=4, two=2)
                        t1, t2 = pv[:, :, :, 0], pv[:, :, :, 1]
                        if n is None:
                            CP("act", dst[:, :, 0:64], t1)
                            CP("act", dst[:, :, 64:128], t2)
                            return
                        cs = COS[:, n, :].un(1).bc([128, 4, 64])
                        sn = SIN[:, n, :].un(1).bc([128, 4, 64])
                        a = tmp_rot.next()
                        b2 = tmp_rot.next()
                        TT("dve", a.v(), t1, cs, ALU.mult)
                        TT("dve", b2.v(), t2, sn, ALU.mult)
                        TT("pool", dst[:, :, 0:64], a.v(), b2.v(), ALU.subtract)
                        c2 = tmp_rot.next()
                        d2 = tmp_rot.next()
                        TT("dve", c2.v(), t1, sn, ALU.mult)
                        TT("dve", d2.v(), t2, cs, ALU.mult)
                        TT("pool", dst[:, :, 64:128], c2.v(), d2.v(), ALU.add)

                    with contextlib.ExitStack() as sc:
                        Wkv = sbt(sc, "Wkv", [128, 8, 1536], BF16)
                        DMA(Wkv.v(), WIN[:, :, 1536:3072])
                        rtmp = rot(sc, "rtmp", [128, 4, 64], F32, 4)
                        krot = rot(sc, "krot", [128, 4, 128], BF16, 2)
                        kf = rot(sc, "kf", [128, 4, 128], BF16, 2)
                        kb = rot(sc, "kb", [128, 4, 128], BF16, 2)
                        vtmp = rot(sc, "vtmp", [128, 1024], BF16, 2)
                        SBrun = sbt(sc, "SBrun", [128, 1024], F32)

                        def s1_a(j, c):
                            n = j - 2
                            pk = PA.next()
                            pv_ = PA.next()
                            for kc in range(8):
                                MM(pk[:, 0:512], hT[j][:, kc, :], Wkv[:, kc, 0:512], start=(kc == 0), stop=(kc == 7), inc=(kc == 7))
                            for hf in range(2):
                                for kc in range(8):
                                    MM(pv_[:, hf * 512:(hf + 1) * 512], hT[j][:, kc, :], Wkv[:, kc, 512 + hf * 512:1024 + hf * 512],
                                       start=(kc == 0), stop=(kc == 7), inc=(kc == 7))
                            vt = VS[n] if n >= 0 else vtmp.next()
                            CP("act", vt.v(), pv_.v())
                            kr = KR[n] if n >= 0 else krot.next()
                            rope(pk[:, 0:512], kr, n if n >= 0 else None, rtmp)
                            c["vt"], c["kr"] = vt, kr
                            if n < 0:
                                c["kft"] = kf.next()
                                TT("pool", c["kft"].v(), kr.v(), DF.v().un(2).bc([128, 4, 128]), ALU.mult)
                            if n != 0:
                                c["kbt"] = kb.next()
                                TT("pool", c["kbt"].v(), kr.v(), DB.v().un(2).bc([128, 4, 128]), ALU.mult)

                        def s1_b(j, c):
                            n = j - 2
                            vt, kr = c["vt"], c["kr"]
                            if n >= 0:
                                pt = pt_next()
                                for h in range(4):
                                    TR(pt[:, h * 128:(h + 1) * 128], kr[:, h, :], identb.v(), inc=(h == 3))
                                CP("act", kT[n].v(), pt[:, 0:512].re("p (h t) -> p h t", h=4))
                            if n < 0:
                                kft = c["kft"]
                                pf = PA.next()
                                for h in range(4):
                                    MM(pf[:, h * 256:(h + 1) * 256], kft[:, h, :], vt[:, h * 256:(h + 1) * 256], start=True, stop=True, inc=(h == 3))
                                if j == 0:
                                    CP("act", SFrun.v(), pf.v())
                                else:
                                    for h in range(4):
                                        sl = slice(h * 256, (h + 1) * 256)
                                        STT(SFrun[:, sl], SFrun[:, sl], G128F[:, h:h + 1], pf[:, sl], ALU.mult, ALU.add)
                            if n != 0:
                                kbt = c["kbt"]
                                pb = PA.next()
                                for h in range(4):
                                    MM(pb[:, h * 256:(h + 1) * 256], kbt[:, h, :], vt[:, h * 256:(h + 1) * 256], start=True, stop=True, inc=(h == 3))
                                if j == 0:
                                    CP("act", SBrun.v(), pb.v())
                                elif j == 1:
                                    for h in range(4):
                                        sl = slice(h * 256, (h + 1) * 256)
                                        STT(SBrun[:, sl], pb[:, sl], G128B[:, h:h + 1], SBrun[:, sl], ALU.mult, ALU.add)
                                    CP("act", SB[15].v(), SBrun.v())
                                else:
                                    CP("act", SB[n - 1].v(), pb.v())

                        pipeline(18, [s1_a, s1_b])
                        for n in range(15, 0, -1):
                            for h in range(4):
                                sl = slice(h * 256, (h + 1) * 256)
                                STT(SBrun[:, sl], SBrun[:, sl], G128B[:, h:h + 1], SB[n - 1][:, sl], ALU.mult, ALU.add)
                            CP("act", SB[n - 1].v(), SBrun.v())
                        S.barrier()
                        _chk("s1")

                    with contextlib.ExitStack() as sc:
                        Wq = sbt(sc, "Wq", [128, 8, 512], BF16)
                        DMA(Wq.v(), WIN[:, :, 1024:1536])
                        rtmp = rot(sc, "rtmp2", [128, 4, 64], F32, 4)
                        kf = rot(sc, "kf2", [128, 4, 128], BF16, 4)
                        SFb = rot(sc, "SFb", [128, 1024], BF16, 2)
                        qrot = rot(sc, "qrot", [128, 4, 128], BF16, 2)
                        CFT = sbt(sc, "CFT", [128, 4, 128], F32)
                        CBT = sbt(sc, "CBT", [128, 4, 128], F32)
                        dti = sbt(sc, "dti", [128, 4, 128], I32)
                        for (tab, lg, step, base) in ((CFT, lgf, 1, 1), (CBT, lgb, -1, 128)):
                            S.op("pool", lambda e, step=step, base=base: e.iota(dti.ap, pattern=[[0, 4], [step, 128]], base=base, channel_multiplier=0),
                                 writes=[dti])
                            CP("dve", tab.v(), dti.v())
                            TT("dve", tab.v(), tab.v(), lg.v().un(2).bc([128, 4, 128]), ALU.mult)
                            ACT(tab.v(), tab.v(), AF.Exp)
                        qT3 = rot(sc, "qT3", [128, 3, 4, 128], BF16, 3)
                        Pm = rot(sc, "Pm", [128, 4, 128], BF16, 3)

                        porot = Rot([PA.ts[1], PA.ts[2]])
                        pfrot = Rot([PA.ts[0]])

                        def s2a_a(n, c):
                            j = n + 2
                            pq = PTf.next()
                            for kc in range(8):
                                MM(pq[:, 0:512], hT[j][:, kc, :], Wq[:, kc, :], start=(kc == 0), stop=(kc == 7), inc=(kc == 7))
                            qr = qrot.next()
                            rope(pq[:, 0:512], qr, n, rtmp)
                            c["qr"] = qr
                            if n < NCH - 1:
                                c["kft"] = kf.next()
                                TT("pool", c["kft"].v(), KR[n].v(), DF.v().un(2).bc([128, 4, 128]), ALU.mult)

                        def s2a_b(n, c):
                            qr = c["qr"]
                            qt = qT3.next()
                            pt = pt_next()
                            for h in range(4):
                                TR(pt[:, h * 128:(h + 1) * 128], qr[:, h, :], identb.v(), inc=(h == 3))
                            CP("act", qt[:, 0, :, :], pt[:, 0:512].re("p (h t) -> p h t", h=4))
                            TT("dve", qt[:, 1, :, :], qt[:, 0, :, :], CFT.v(), ALU.mult)
                            TT("pool", qt[:, 2, :, :], qt[:, 0, :, :], CBT.v(), ALU.mult)
                            c["qt"] = qt

                        def s2a_c(n, c):
                            qt = c["qt"]
                            pa_ = PTf.next()
                            for h in range(4):
                                MM(pa_[:, h * 128:(h + 1) * 128], kT[n][:, h, :], qt[:, 0, h, :], start=True, stop=True, inc=(h == 3))
                            pm = Pm.next()
                            TT("dve", pm.v(), pa_[:, 0:512].re("p (h t) -> p h t", h=4), MASKT.v(), ALU.mult)
                            c["pm"] = pm

                        sfb_cur = [SFb.next()]
                        CP("act", sfb_cur[0].v(), SFrun.v())

                        def s2a_d(n, c):
                            qt, pm = c["qt"], c["pm"]
                            sfb = sfb_cur[0]
                            po = porot.next()
                            for h in range(4):
                                sl = slice(h * 256, (h + 1) * 256)
                                MM(po[:, sl], pm[:, h, :], VS[n][:, sl], start=True, stop=False, inc=False)
                                MM(po[:, sl], qt[:, 1, h, :], sfb[:, sl], start=False, stop=False, inc=False)
                                MM(po[:, sl], qt[:, 2, h, :], SB[n][:, sl], start=False, stop=True, inc=(h == 3))
                            if n < NCH - 1:
                                kft = c["kft"]
                                pf = pfrot.next()
                                for h in range(4):
                                    MM(pf[:, h * 256:(h + 1) * 256], kft[:, h, :], VS[n][:, h * 256:(h + 1) * 256], start=True, stop=True, inc=(h == 3))
                                for h in range(4):
                                    sl = slice(h * 256, (h + 1) * 256)
                                    STT(SFrun[:, sl], SFrun[:, sl], G128F[:, h:h + 1], pf[:, sl], ALU.mult, ALU.add)
                            st = st_rot.next()
                            mv = mv_rot.next()
                            for h in range(4):
                                S.op("dve", lambda e, h=h: e.bn_stats(out=st.ap[:, h, :], in_=po.ap[:, h * 256:(h + 1) * 256]), reads=[po], writes=[st])
                            for h in range(4):
                                S.op("dve", lambda e, h=h: e.bn_aggr(out=mv.ap[:, h, :], in_=st.ap[:, h, :]), reads=[st], writes=[mv])
                            rs = rs_rot.next()
                            TS("dve", rs[:, 0:4], mv[:, :, 1], EPS, ALU.add)
                            TT("pool", rs[:, 0:4], rs[:, 0:4], nhalf.v(), ALU.pow)
                            TT("pool", rs[:, 4:8], mv[:, :, 0], rs[:, 0:4], ALU.mult)
                            TS("pool", rs[:, 4:8], rs[:, 4:8], -1.0, ALU.mult, 1.0, ALU.mult)
                            c["po"], c["rs"] = po, rs

                        def s2a_d2(n, c):
                            if n < NCH - 1:
                                sfb_cur[0] = SFb.next()
                                CP("dve", sfb_cur[0].v(), SFrun.v())

                        def s2a_e(n, c):
                            po, rs = c["po"], c["rs"]
                            for h in range(4):
                                sl = slice(h * 256, (h + 1) * 256)
                                ACT(VS[n][:, sl], po[:, sl], AF.Identity, bias=rs[:, 4 + h:5 + h], scale=rs[:, h:h + 1])

                        pipeline(NCH, [(0, s2a_a), (1, s2a_b), (2, s2a_c), (3, s2a_d), (4, s2a_e), (3, s2a_d2)])
                        S.barrier()
                        _chk("s2a")

                with contextlib.ExitStack() as sc:
                    Wgr = sbt(sc, "Wgr", [128, 8, 1024], BF16)
                    Wgb = sbt(sc, "Wgb", [128, 8, 1024], BF16)
                    Wro = sbt(sc, "Wro", [128, 8, 1024], BF16)
                    DMA(Wgr.v(), WIN[:, :, 3072:4096])
                    DMA(Wgb.v(), WIN[:, :, 5120:6144])
                    DMA(Wro.v(), WRO.v())
                    sg = rot(sc, "sg", [128, 1024], F32, 2)
                    ybin = rot(sc, "ybin", [128, 1024], BF16, 3)
                    ybT = rot(sc, "ybT", [128, 8, 128], BF16, 3)
                    sgb = rot(sc, "sgb", [128, 1024], F32, 4)

                    def s2b_a(n, c):
                        j = n + 2
                        pg = PA.next()
                        mm_wide(pg, lambda kc: hT[j][:, kc, :], Wgr, 8)
                        s1 = sg.next()
                        ACT(s1.v(), pg.v(), AF.Silu)
                        yb = ybin.next()
                        TT("pool", yb.v(), s1.v(), VS[n].v(), ALU.mult)
                        pgb = PA.next()
                        mm_wide(pgb, lambda kc: hT[j][:, kc, :], Wgb, 8)
                        s2 = sgb.next()
                        ACT(s2.v(), pgb.v(), AF.Sigmoid)
                        c["yb"], c["s2"] = yb, s2

                    def s2b_b(n, c):
                        ybt = ybT.next()
                        transposeN(ybt.v(), c["yb"].v(), 8)
                        c["ybt"] = ybt

                    def s2b_c(n, c):
                        ybt = c["ybt"]
                        py = PA.next()
                        mm_wide(py, lambda kc: ybt[:, kc, :], Wro, 8)
                        TT("dve", VS[n].v(), py.v(), c["s2"].v(), ALU.mult)

                    pipeline(NCH, [s2b_a, s2b_b, s2b_c])
                    S.barrier()
                    _chk("s2b")

                with contextlib.ExitStack() as sCv:
                    cT_all = sbt(sCv, "cT", [128, 4, L], BF16)
                    cT = [T(cT_all.ap[:, :, n * 128:(n + 1) * 128]) for n in range(NCH)]
                    with contextlib.ExitStack() as sY:
                        yT = sbt(sY, "yT", [128, 4, L + 30], BF16)
                        DGa = sbt(sY, "DG", [128, 4, 31, 128], BF16)
                        DG = [T(DGa.ap[:, cc, :, :]) for cc in range(4)]
                        MSET("pool", yT[:, :, 0:15], 0.0)
                        MSET("pool", yT[:, :, L + 15:L + 30], 0.0)
                        for cc in range(4):
                            for k in range(31):
                                TS("pool" if cc % 2 else "dve", DG[cc][:, k, :], identf.v(), cwT[:, cc, k:k + 1], ALU.mult, 1.0, ALU.mult)
                        with contextlib.ExitStack() as sc:
                            Wu = sbt(sc, "Wu", [128, 8, 1024], BF16)
                            DMA(Wu.v(), WIN[:, :, 0:1024])
                            sgu = rot(sc, "sgu", [128, 512], F32, 2)
                            yg = rot(sc, "yg", [128, 512], BF16, 3)

                            def c1_a(n, c):
                                j = n + 2
                                pu = PA.next()
                                mm_wide(pu, lambda kc: hT[j][:, kc, :], Wu, 8)
                                s1 = sgu.next()
                                ACT(s1.v(), pu[:, 512:1024], AF.Sigmoid)
                                y1 = yg.next()
                                TT("dve", y1.v(), pu[:, 0:512], s1.v(), ALU.mult)
                                c["y1"] = y1

                            def c1_b(n, c):
                                y1 = c["y1"]
                                pt = pt_next()
                                for cc in range(4):
                                    TR(pt[:, cc * 128:(cc + 1) * 128], y1[:, cc * 128:(cc + 1) * 128], identb.v(), inc=(cc == 3))
                                CP("act", yT[:, :, 15 + n * 128:15 + (n + 1) * 128], pt[:, 0:512].re("p (c t) -> p c t", c=4))

                            pipeline(NCH, [c1_a, c1_b])
                            S.barrier()
                            _chk("c1")
                        with contextlib.ExitStack() as sc:
                            cf = rot(sc, "cf", [128, 4, 512], F32, 2)
                            cs_ = rot(sc, "cs", [128, 512], BF16, 3)
                            cfts = {}

                            pcrot = Rot([T(PA.ts[0].ap[:, 0:512]), T(PA.ts[0].ap[:, 512:1024])])
                            plrot = Rot([T(PA.ts[1].ap[:, 0:512]), T(PA.ts[1].ap[:, 512:1024]),
                                         T(PA.ts[2].ap[:, 0:512]), T(PA.ts[2].ap[:, 512:1024])])

                            def c2_a(n, c):
                                tg, cc = n // 4, n % 4
                                if cc == 0:
                                    cfts[tg] = cf.next()
                                cft = cfts[tg]
                                pc = pcrot.next()
                                for k in range(31):
                                    MM(pc.v(), DG[cc][:, k, :], yT[:, cc, tg * 512 + k:tg * 512 + k + 512],
                                       start=(k == 0), stop=(k == 30), inc=(k == 30))
                                ACT(cft[:, cc, :], pc.v(), AF.Identity, bias=cwT[:, cc, 31:32])

                            def c2_b(n, c):
                                cft = cfts[n // 4]
                                i = n % 4
                                pl = plrot.next()
                                for cc in range(4):
                                    TR(pl[:, cc * 128:(cc + 1) * 128], cft[:, cc, i * 128:(i + 1) * 128], identf.v(), inc=(cc == 3))
                                c["rs"] = ln_stats(pl.v(), 512)
                                c["pl"] = pl

                            def c2_c(n, c):
                                c2 = cs_.next()
                                ln_apply(c2.v(), c["pl"].v(), c["rs"])
                                c["c2"] = c2

                            def c2_d(n, c):
                                c2 = c["c2"]
                                pt = pt_next()
                                for cc in range(4):
                                    TR(pt[:, cc * 128:(cc + 1) * 128], c2[:, cc * 128:(cc + 1) * 128], identb.v(), inc=(cc == 3))
                                for cc in range(4):
                                    ACT(cT[n][:, cc, :], pt[:, cc * 128:(cc + 1) * 128], AF.Silu, bias=clnT[:, cc, 1:2], scale=clnT[:, cc, 0:1])

                            pipeline(NCH, [(0, c2_a), (4, c2_b), (5, c2_c), (6, c2_d)])
                            S.barrier()
                            _chk("c2")
                    with contextlib.ExitStack() as sc:
                        Wga = sbt(sc, "Wga", [128, 8, 1024], BF16)
                        Wco = sbt(sc, "Wco", [128, 4, 1024], BF16)
                        Wout = sbt(sc, "Wout", [128, 8, 1024], BF16)
                        DMA(Wco.v(), WCO.v())
                        DMA(Wga.v(), WIN[:, :, 4096:5120])
                        DMA(Wout.v(), WOUT.v())
                        L1G = load_bc(sc, "L1G", ln1_g_d, 0, 0)
                        L1B = load_bc(sc, "L1B", ln1_b_d, 0, 0)
                        SH2 = load_bc(sc, "SH2", MOD, b, 3072)
                        SC2 = load_bc(sc, "SC2", MOD, b, 4096)
                        sga = rot(sc, "sga", [128, 1024], F32, 1)
                        G1 = sga.ts[0]
                        DMA(G1.v(), V(MOD, MOD.ap[b:b + 1, 2048:3072].partition_broadcast(128)))
                        for kc in range(8):
                            TT("pool", Wout[:, kc, :], Wout[:, kc, :], G1.v(), ALU.mult)
                        mb = rot(sc, "mb", [128, 1024], BF16, 2)
                        mT = rot(sc, "mT", [128, 8, 128], BF16, 2)
                        xin = rot(sc, "xin3", [128, 1024], F32, 3)
                        rr = rot(sc, "rr", [128, 1024], F32, 4)
                        h2b = rot(sc, "h2b", [128, 1024], BF16, 2)
                        h2T = rot(sc, "h2T", [128, 8, 128], BF16, 2)
                        sm = rot(sc, "sm", [128, 20], F32, 3)

                        def p3_a(n, c):
                            j = n + 2
                            xi = xin.next()
                            DMA(xi.v(), x_d[b, n * 128:(n + 1) * 128, :])
                            pya = PA.next()
                            mm_wide(pya, lambda kc: cT[n][:, kc, :], Wco, 4)
                            pga = PA.next()
                            mm_wide(pga, lambda kc: hT[j][:, kc, :], Wga, 8)
                            s1 = sga.next()
                            ACT(s1.v(), pga.v(), AF.Sigmoid)
                            c["xi"], c["s1"], c["pya"] = xi, s1, pya

                        def p3_a2(n, c):
                            s1 = c["s1"]
                            TT("dve", s1.v(), c["pya"].v(), s1.v(), ALU.mult)
                            m2 = mb.next()
                            TT("pool", m2.v(), s1.v(), VS[n].v(), ALU.add)
                            c["m2"] = m2

                        def p3_b(n, c):
                            mt = mT.next()
                            transposeN(mt.v(), c["m2"].v(), 8)
                            c["mt"] = mt

                        def p3_c(n, c):
                            mt = c["mt"]
                            pyo = PA.next()
                            mm_wide(pyo, lambda kc: mt[:, kc, :], Wout, 8)
                            c["pyo"] = pyo

                        def p3_c2(n, c):
                            pyo = c["pyo"]
                            r1 = rr.next()
                            STT(r1.v(), c["xi"].v(), ALPHA, pyo.v(), ALU.mult, ALU.add)
                            c["rs1"] = ln_stats(r1.v(), 1024)
                            c["xo"] = r1

                        def p3_d(n, c):
                            xo = c["xo"]
                            ln_apply(xo.v(), xo.v(), c["rs1"])
                            TT("dve", xo.v(), xo.v(), L1G.v(), ALU.mult)
                            TT("pool", xo.v(), xo.v(), L1B.v(), ALU.add)
                            DMA(ACC[b][n * 128:(n + 1) * 128, :], xo.v())
                            if debug:
                                DMA(DBG_X1[b * L + n * 128:b * L + (n + 1) * 128, :], xo.v())

                        def p3_d2(n, c):
                            c["rs2"] = ln_stats(c["xo"].v(), 1024)

                        def p3_e(n, c):
                            xo = c["xo"]
                            ln_apply(xo.v(), xo.v(), c["rs2"])
                            TT("dve", xo.v(), xo.v(), SC2.v(), ALU.mult)
                            hb_ = h2b.next()
                            TT("pool", hb_.v(), xo.v(), SH2.v(), ALU.add)
                            DMA(H2[b * L + n * 128:b * L + (n + 1) * 128, :], hb_.v())
                            c["hb"] = hb_

                        def p3_f(n, c):
                            ht = h2T.next()
                            transposeN(ht.v(), c["hb"].v(), 8)
                            c["ht"] = ht

                        def p3_g(n, c):
                            ht = c["ht"]
                            pr = PTf.next()
                            for kc in range(8):
                                MM(pr[:, 0:NE], ht[:, kc, :], WR[:, kc, :], start=(kc == 0), stop=(kc == 7), inc=(kc == 7))
                            s_ = sm.next()
                            S.op("dve", lambda e, s_=s_, pr=pr: e.tensor_reduce(out=s_.ap[:, 16:17], in_=pr.ap[:, 0:NE], axis=mybir.AxisListType.X, op=ALU.max),
                                 reads=[pr], writes=[s_])
                            TS("dve", s_[:, 17:18], s_[:, 16:17], -1.0, ALU.mult)
                            ACT(s_[:, 0:16], pr[:, 0:NE], AF.Exp, bias=s_[:, 17:18])
                            S.op("dve", lambda e, s_=s_: e.tensor_reduce(out=s_.ap[:, 18:19], in_=s_.ap[:, 0:16], axis=mybir.AxisListType.X, op=ALU.add),
                                 reads=[s_], writes=[s_])
                            S.op("dve", lambda e, s_=s_: e.reciprocal(out=s_.ap[:, 19:20], in_=s_.ap[:, 18:19]), reads=[s_], writes=[s_])
                            TS("dve", AFF[:, n, b * NE:(b + 1) * NE], s_[:, 0:16], s_[:, 19:20], ALU.mult)

                        pipeline(NCH, [(4, p3_d2), (5, p3_e), (3, p3_d), (7, p3_g), (6, p3_f), (0, p3_a), (1, p3_b), (2, p3_c), (0, p3_a2), (2, p3_c2)])
                        S.barrier()
                        _chk("p3")

        IDXT = sbt(P, "IDXT", [128, 2, NS * NE], I32)
        GT = sbt(P, "GT", [128, 2, NS * NE], F32)
        with contextlib.ExitStack() as sc:
            G2 = [sbt(sc, "G2_%d" % b, [128, 1024], F32) for b in range(NS)]
            for b in range(NS):
                DMA(G2[b].v(), V(MOD, MOD.ap[b:b + 1, 5120:6144].partition_broadcast(128)))
                TS("pool", G2[b].v(), G2[b].v(), 1.0 / ALPHA, ALU.mult, 1.0, ALU.mult)
            wbf = [[sbt(sc, "wbf%d_%d" % (m, i), [128, 8, 1024], BF16) for i in range(2)] for m in range(3)]
            wbfp = [[[T(wbf[m][i].ap[:, 2 * q:2 * q + 2, :]) for q in range(4)] for i in range(2)] for m in range(3)]
            wst = rot(sc, "wst", [128, 2, 1024], F32, 2)
            wsrc = (w_gate_d, w_up_d, w_down_d)
            castn = [0]
            cast_engs = ["pool", "act"]

            def weight_pieces(e_):
                buf = e_ % 2
                fns = []
                for m in range(3):
                    wv = wsrc[m].v()[e_].re("(c p) f -> p c f", p=128)
                    for q in range(4):
                        def f(m=m, q=q, wv=wv):
                            st_ = wst.next()
                            DMA(st_.v(), wv[:, 2 * q:2 * q + 2, :])
                            CP(cast_engs[castn[0] % len(cast_engs)], wbfp[m][buf][q].v(), st_.v())
                            castn[0] += 1
                        fns.append(f)
                return fns

            def gather(e_, g):
                b, half = g // 2, g % 2
                xg = xe.next()
                col = b * NE + e_
                S.dma("pool", lambda e, xg=xg, half=half, col=col: e.indirect_dma_start(
                    out=xg.ap, out_offset=None, in_=H2.ap,
                    in_offset=bass.IndirectOffsetOnAxis(ap=IDXT.ap[:, half, col:col + 1], axis=0)),
                    reads=[H2, IDXT], writes=[xg])
                return xg

            for f in weight_pieces(0):
                f()
            cast_engs[:] = ["act", "act", "dve"]
            with contextlib.ExitStack() as scB:
                AT = sbt(scB, "AT", [64, L], F32)
                vals = sbt(scB, "vals", [64, CAP], F32)
                idxu = sbt(scB, "idxu", [64, CAP], U32)
                idxf = sbt(scB, "idxf", [64, CAP], F32)
                for n0 in range(0, NCH, 4):
                    pa = PA.next()
                    for i in range(4):
                        TR(pa[0:64, i * 128:(i + 1) * 128], AFF[:, n0 + i, :], identf.v(), inc=(i == 3))
                    CP("act", AT[:, n0 * 128:(n0 + 4) * 128], pa[0:64, 0:512])
                for it in range(CAP // 8):
                    sl = slice(it * 8, (it + 1) * 8)
                    S.op("dve", lambda e, sl=sl: e.max(out=vals.ap[:, sl], in_=AT.ap), reads=[AT], writes=[vals])
                    S.op("dve", lambda e, sl=sl: e.max_index(out=idxu.ap[:, sl], in_max=vals.ap[:, sl], in_values=AT.ap), reads=[AT, vals], writes=[idxu])
                    S.op("dve", lambda e, sl=sl: e.match_replace(out=AT.ap, in_to_replace=vals.ap[:, sl], in_values=AT.ap, imm_value=-1.0),
                         reads=[AT, vals], writes=[AT])
                pci = sbt(scB, "pci", [64, 1], I32)
                pcf = sbt(scB, "pcf", [64, 1], F32)
                offs = sbt(scB, "offs", [64, 1], F32)
                tmpo = sbt(scB, "tmpo", [64, 1], F32)
                S.op("pool", lambda e: e.iota(pci.ap, pattern=[[0, 1]], base=0, channel_multiplier=1), writes=[pci])
                CP("dve", pcf.v(), pci.v())
                TS("dve", offs.v(), pcf.v(), 16.0, ALU.is_ge)
                for thr in (32.0, 48.0):
                    TS("dve", tmpo.v(), pcf.v(), thr, ALU.is_ge)
                    TT("dve", offs.v(), offs.v(), tmpo.v(), ALU.add)
                TS("dve", offs.v(), offs.v(), float(L), ALU.mult)
                CP("dve", idxf.v(), idxu.v())
                TS("dve", idxf.v(), idxf.v(), offs.v(), ALU.add)
                TS("dve", idxf.v(), idxf.v(), 0.0, ALU.max, float(NS * L - 1), ALU.min)
                for half in range(2):
                    pa = PA.next()
                    TR(pa[:, 0:64], idxf[:, half * 128:(half + 1) * 128], identf[0:64, 0:64], inc=True)
                    CP("dve", IDXT[:, half, :], pa[:, 0:64])
                    pa2 = PA.next()
                    TR(pa2[:, 0:64], vals[:, half * 128:(half + 1) * 128], identf[0:64, 0:64], inc=True)
                    CP("dve", GT[:, half, :], pa2[:, 0:64])
                S.barrier()

            if debug:
                DMA(DBG_IDX.v(), IDXT.v())
                DMA(DBG_GT.v(), GT.v())
                DMA(DBG_AFF.v(), AFF.v())
            xe = rot(sc, "xe", [128, 1024], BF16, 8)
            xeT = sbt(sc, "xeT", [128, 8, 1024], BF16)
            xeTg = [T(xeT.ap[:, :, g * 128:(g + 1) * 128]) for g in range(8)]
            heT = sbt(sc, "heT", [128, 8, 1024], BF16)
            heTs = [[T(heT.ap[:, fc, sg * 512:(sg + 1) * 512]) for sg in range(2)] for fc in range(8)]
            sgl = rot(sc, "sgl", [128, 512], F32, 2)
            ye = rot(sc, "ye", [128, 1024], F32, 2)
            for g in range(8):
                xg = gather(0, g)
                transposeN(xeTg[g].v(), xg.v(), 8)
            for e_ in range(NE):
                buf = e_ % 2
                pieces = weight_pieces(e_ + 1) if e_ + 1 < NE else []
                nxt = []
                for fc in range(8):
                    for sg in range(2):
                        pgu = PA.next()
                        for m in (0, 1):
                            for kc in range(8):
                                S.op("pe", lambda e, m=m, kc=kc, fc=fc, sg=sg, pgu=pgu: e.matmul(
                                    pgu.ap[:, m * 512:(m + 1) * 512], lhsT=wbf[m][buf].ap[:, kc, fc * 128:(fc + 1) * 128],
                                    rhs=xeT.ap[:, kc, sg * 512:(sg + 1) * 512], start=(kc == 0), stop=(kc == 7)),
                                    reads=[wbfp[m][buf][kc // 2]] + xeTg[sg * 4:(sg + 1) * 4], writes=[pgu], inc=(kc == 7))
                        if pieces:
                            pieces.pop(0)()
                        s1 = sgl.next()
                        ACT(s1.v(), pgu[:, 0:512], AF.Silu)
                        TT("dve", heTs[fc][sg].v(), pgu[:, 512:1024], s1.v(), ALU.mult)
                        if e_ + 1 < NE and fc >= 4:
                            nxt.append(gather(e_ + 1, len(nxt)))
                while pieces:
                    pieces.pop(0)()
                for g in range(8):
                    b, half = g // 2, g % 2
                    col = b * NE + e_
                    pd = PA.next()
                    for hf in range(2):
                        for fc in range(8):
                            S.op("pe", lambda e, hf=hf, fc=fc, g=g, pd=pd: e.matmul(
                                pd.ap[:, hf * 512:(hf + 1) * 512], lhsT=heT.ap[:, fc, g * 128:(g + 1) * 128],
                                rhs=wbf[2][buf].ap[:, fc, hf * 512:(hf + 1) * 512], start=(fc == 0), stop=(fc == 7)),
                                reads=[wbfp[2][buf][fc // 2], heTs[fc][g // 4]], writes=[pd], inc=(fc == 7))
                    y1 = ye.next()
                    STT(y1.v(), pd.v(), GT[:, half, col:col + 1], G2[b].v(), ALU.mult, ALU.mult)
                    S.dma("pool", lambda e, y1=y1, half=half, col=col, b=b: e.indirect_dma_start(
                        out=ACCALL, out_offset=bass.IndirectOffsetOnAxis(ap=IDXT.ap[:, half, col:col + 1], axis=0),
                        in_=y1.ap, in_offset=None, compute_op=ALU.add),
                        reads=[y1, IDXT], writes=[ACC[b]])
                    if e_ + 1 < NE:
                        transposeN(xeTg[g].v(), nxt[g].v(), 8)
            S.barrier()

        with contextlib.ExitStack() as sc:
            L2G = sbt(sc, "L2G", [128, 1024], F32)
            L2B = sbt(sc, "L2B", [128, 1024], F32)
            DMA(L2G.v(), V(ln2_g_d, ln2_g_d.ap.partition_broadcast(128)))
            DMA(L2B.v(), V(ln2_b_d, ln2_b_d.ap.partition_broadcast(128)))
            ain = rot(sc, "ain", [128, 1024], F32, 8)
            aout = rot(sc, "aout", [128, 1024], F32, 8)
            def d_l(i, c):
                b, n = i // NCH, i % NCH
                a = ain.next()
                DMA(a.v(), ACC[b][n * 128:(n + 1) * 128, :])
                c["a"] = a

            def d_l2(i, c):
                pass

            def d_a(i, c):
                c["rs"] = ln_stats(c["a"].v(), 1024, eps=EPS / (ALPHA * ALPHA))

            def d_b(i, c):
                o = aout.next()
                ln_apply(o.v(), c["a"].v(), c["rs"])
                TT("dve", o.v(), o.v(), L2G.v(), ALU.mult)
                TT("pool", o.v(), o.v(), L2B.v(), ALU.add)
                c["o"] = o

            def d_c(i, c):
                b, n = i // NCH, i % NCH
                ot = T(out_d.ap[b * L + n * 128:b * L + (n + 1) * 128, :])
                DMA(ot.v(), c["o"].v())

            pipeline(NS * NCH, [(0, d_l), (4, d_a), (5, d_b), (6, d_c)])
            S.barrier()
        print("kernel: n_ins", S.n_ins, {k: v for k, v in S.cnt.items() if not k.startswith("d")})
    return nc


_NC_CACHE = {}


def kernel(x, c, ctx, c_ctx, w_ada, b_ada, w_in, conv_w, conv_b, conv_ln_g, conv_ln_b, w_conv_out,
           log_decay_f, log_decay_b, ret_gn_g, w_ret_out, w_out, ln1_g, ln1_b,
           w_router, w_gate, w_up, w_down, ln2_g, ln2_b):
    f = lambda a: np.ascontiguousarray(np.asarray(a, dtype=np.float32))
    x, c, ctx, c_ctx = f(x), f(c), f(ctx), f(c_ctx)
    n_cores = 8
    if "nc" not in _NC_CACHE:
        _NC_CACHE["nc"] = build_program()
    nc = _NC_CACHE["nc"]
    convw = np.concatenate([f(conv_w)[0], f(conv_b)[0][None, :]], axis=0)
    shared = {
        "w_ada": f(w_ada)[0], "b_ada": f(b_ada), "w_in": f(w_in)[0], "convw": f(convw),
        "conv_ln_g": f(conv_ln_g), "conv_ln_b": f(conv_ln_b), "w_conv_out": f(w_conv_out)[0],
        "log_decay_f": f(log_decay_f), "log_decay_b": f(log_decay_b), "ret_gn_g": f(ret_gn_g),
        "w_ret_out": f(w_ret_out)[0], "w_out": f(w_out)[0], "ln1_g": f(ln1_g), "ln1_b": f(ln1_b),
        "w_router": f(w_router)[0], "w_gate": f(w_gate)[0], "w_up": f(w_up)[0], "w_down": f(w_down)[0],
        "ln2_g": f(ln2_g), "ln2_b": f(ln2_b),
    }
    in_maps = []
    for i in range(n_cores):
        sl = slice(i * NS, (i + 1) * NS)
        cc = np.concatenate([c[sl], c_ctx[None, :]], axis=0)
        cT = np.ascontiguousarray(cc.T.reshape(8, 128, 5).transpose(1, 0, 2))
        m = dict(shared)
        m["x"] = np.ascontiguousarray(x[sl])
        m["ctx"] = np.ascontiguousarray(ctx[sl])
        m["cT"] = cT
        in_maps.append(m)
    res = run_bass_kernel_spmd(nc, in_maps, core_ids=list(range(n_cores)))
    outs = [np.asarray(r["out"]).reshape(NS, L, D) for r in res.results]
    return np.concatenate(outs, axis=0).astype(np.float32)
```

```python
import contextlib
import math
import os
import numpy as np
import concourse.bass as bass
import concourse.mybir as mybir
from concourse.bass_utils import run_bass_kernel_spmd

dt = mybir.dt
F32, BF16, I32, U32 = dt.float32, dt.bfloat16, dt.int32, dt.uint32
AF = mybir.ActivationFunctionType
ALU = mybir.AluOpType

NS = 4
L = 2048
D = 1024
NCH = 16
ALPHA = 2.0 ** 0.25
EPS = 1e-5
DKS = 128.0 ** -0.5
NE = 16
CAP = 256


class V:
    def __init__(self, t, ap):
        self.t, self.ap = t, ap

    def __getitem__(self, k):
        return V(self.t, self.ap[k])

    def re(self, s, **kw):
        return V(self.t, self.ap.rearrange(s, **kw))

    def bc(self, shape):
        return V(self.t, self.ap.to_broadcast(shape))

    def un(self, ax):
        return V(self.t, self.ap.unsqueeze(ax))

    def bitcast(self, d):
        return V(self.t, self.ap.bitcast(d))


class T:
    def __init__(self, ap):
        self.ap = ap
        self.w = None
        self.r = {}

    def __getitem__(self, k):
        return V(self, self.ap[k])

    def v(self):
        return V(self, self.ap)


class Rot:
    def __init__(self, ts):
        self.ts = ts
        self.i = 0

    def next(self):
        t = self.ts[self.i % len(self.ts)]
        self.i += 1
        t.gen = getattr(t, "gen", 0) + 1
        return t


class _Stop(Exception):
    pass


def _chk(name):
    if os.environ.get("KSTOP") == name:
        raise _Stop()


class Sched:
    N_DMA_SEMS = 40

    def __init__(self, nc, es):
        self.nc = nc
        self.eng = {"pe": nc.tensor, "act": nc.scalar, "dve": nc.vector, "pool": nc.gpsimd, "sp": nc.sync}
        self.semobj = {}
        self.cnt = {}
        for k in self.eng:
            self.semobj[k] = es.enter_context(nc.semaphore("s_" + k))
            self.cnt[k] = 0
        self.seen = {k: {} for k in self.eng}
        self.dma_keys = []
        for i in range(self.N_DMA_SEMS):
            key = "d%d" % i
            self.semobj[key] = es.enter_context(nc.semaphore("s_" + key))
            self.cnt[key] = 0
            self.dma_keys.append(key)
        self.dma_rr = 0
        self.n_ins = 0

    def _collect(self, ek, reads, writes):
        waits = {}

        def need(dep):
            if dep is None:
                return
            key, val = dep
            if waits.get(key, 0) < val:
                waits[key] = val

        for t in reads:
            need(t.w)
        for t in writes:
            if t.w is not None and t.w[0] != ek:
                need(t.w)
            for k, v in t.r.items():
                if k != ek:
                    need((k, v))
        return waits

    def _emit_waits(self, ek, waits):
        E = self.eng[ek]
        for key, val in waits.items():
            if self.seen[ek].get(key, 0) >= val:
                continue
            E.wait_ge(self.semobj[key], val)
            self.seen[ek][key] = val
            self.n_ins += 1

    def op(self, ek, fn, reads=(), writes=(), inc=True):
        waits = self._collect(ek, reads, writes)
        self._emit_waits(ek, waits)
        ins = fn(self.eng[ek])
        self.n_ins += 1
        if inc:
            self.cnt[ek] += 1
            ins.then_inc(self.semobj[ek], 1)
            c = self.cnt[ek]
        else:
            c = self.cnt[ek] + 1
        for t in reads:
            t.r[ek] = c
        for t in writes:
            t.w = (ek, c)
            t.r = {}
        return ins

    def dma(self, q, fn, reads=(), writes=()):
        key = self.dma_keys[self.dma_rr]
        self.dma_rr = (self.dma_rr + 1) % len(self.dma_keys)
        waits = self._collect(key, reads, writes)
        if self.cnt[key] > 0 and waits.get(key, 0) < self.cnt[key]:
            waits[key] = self.cnt[key]
        self._emit_waits(q, waits)
        ins = fn(self.eng[q])
        self.n_ins += 1
        self.cnt[key] += 16
        ins.then_inc(self.semobj[key], 16)
        c = self.cnt[key]
        for t in reads:
            t.r[key] = c
        for t in writes:
            t.w = (key, c)
            t.r = {}
        return ins

    def barrier(self, engines=("pe", "act", "dve", "pool", "sp")):
        for ek in engines:
            waits = {}
            for k, c in self.cnt.items():
                if k != ek and c > 0:
                    waits[k] = c
            self._emit_waits(ek, waits)


def build_program(debug=False):
    holder = {}
    try:
        return _build_inner(debug, holder)
    except _Stop:
        return holder["nc"]


def _build_inner(debug, holder):
    nc = bass.Bass("TRN2", target_bir_lowering=False)
    holder["nc"] = nc

    def din(name, shape, dtype=F32):
        return T(nc.dram_tensor(name, list(shape), dtype, kind="ExternalInput").ap())

    x_d = din("x", [NS, L, D])
    ctx_d = din("ctx", [NS, 256, D])
    cT_d = din("cT", [128, 8, 5])
    w_ada_d = din("w_ada", [D, 6 * D])
    b_ada_d = din("b_ada", [1, 6 * D])
    w_in_d = din("w_in", [D, 6 * D])
    convw_d = din("convw", [32, 512])
    cln_g_d = din("conv_ln_g", [1, 512])
    cln_b_d = din("conv_ln_b", [1, 512])
    w_co_d = din("w_conv_out", [512, D])
    lgf_d = din("log_decay_f", [1, 4])
    lgb_d = din("log_decay_b", [1, 4])
    gn_g_d = din("ret_gn_g", [1, D])
    w_ro_d = din("w_ret_out", [D, D])
    w_out_d = din("w_out", [D, D])
    ln1_g_d = din("ln1_g", [1, D])
    ln1_b_d = din("ln1_b", [1, D])
    w_r_d = din("w_router", [D, NE])
    w_gate_d = din("w_gate", [NE, D, D])
    w_up_d = din("w_up", [NE, D, D])
    w_down_d = din("w_down", [NE, D, D])
    ln2_g_d = din("ln2_g", [1, D])
    ln2_b_d = din("ln2_b", [1, D])
    out_d = T(nc.dram_tensor("out", [NS * L, D], F32, kind="ExternalOutput").ap())

    dk = dict(kind="ExternalOutput") if debug else {}
    MOD = T(nc.dram_tensor("mod_s", [5, 6 * D], F32, **dk).ap())
    WIN = T(nc.dram_tensor("win_s", [128, 8, 6 * D], BF16).ap())
    WRO = T(nc.dram_tensor("wro_s", [128, 8, D], BF16).ap())
    WOUT = T(nc.dram_tensor("wout_s", [128, 8, D], BF16).ap())
    WCO = T(nc.dram_tensor("wco_s", [128, 4, D], BF16).ap())
    H2 = T(nc.dram_tensor("h2_s", [NS * L, D], BF16, **dk).ap())
    ACCALL = nc.dram_tensor("acc_s", [NS * L, D], F32, **dk).ap()
    ACC = [T(ACCALL[b * L:(b + 1) * L, :]) for b in range(NS)]
    if debug:
        DBG_IDX = T(nc.dram_tensor("dbg_idx", [128, 2, NS * NE], I32, kind="ExternalOutput").ap())
        DBG_GT = T(nc.dram_tensor("dbg_gt", [128, 2, NS * NE], F32, kind="ExternalOutput").ap())
        DBG_AFF = T(nc.dram_tensor("dbg_aff", [128, NCH, NS * NE], F32, kind="ExternalOutput").ap())
        DBG_X1 = T(nc.dram_tensor("dbg_x1", [NS * L, D], F32, kind="ExternalOutput").ap())
    dbg = {}

    es = contextlib.ExitStack()
    with es:
        S = Sched(nc, es)

        uniq = [0]

        def sbt(sc, name, shape, dtype):
            uniq[0] += 1
            return T(sc.enter_context(nc.sbuf_tensor("%s_u%d" % (name, uniq[0]), list(shape), dtype))[:])

        def rot(sc, name, shape, dtype, n):
            return Rot([sbt(sc, "%s%d" % (name, i), shape, dtype) for i in range(n)])

        def rd(*vs):
            return [v.t for v in vs if isinstance(v, V)]

        def apof(x):
            return x.ap if isinstance(x, V) else x

        def ACT(out, in_, func, bias=None, scale=None, accum=None):
            kw = {}
            if bias is not None:
                kw["bias"] = apof(bias)
            if scale is not None:
                kw["scale"] = apof(scale)
            if accum is not None:
                kw["accum_out"] = accum.ap
            S.op("act", lambda e: e.activation(out=out.ap, in_=in_.ap, func=func, **kw),
                 reads=rd(in_, bias, scale), writes=[out.t] + ([accum.t] if accum is not None else []))

        def TT(ek, out, a, b, op):
            S.op(ek, lambda e: e.tensor_tensor(out=out.ap, in0=a.ap, in1=b.ap, op=op), reads=rd(a, b), writes=[out.t])

        def TS(ek, out, a, s1, op0, s2=None, op1=None):
            kw = {}
            if op1 is not None:
                kw["op1"] = op1
            S.op(ek, lambda e: e.tensor_scalar(out=out.ap, in0=a.ap, scalar1=apof(s1), scalar2=apof(s2), op0=op0, **kw),
                 reads=rd(a, s1, s2), writes=[out.t])

        def STT(out, a, s, b, op0, op1):
            S.op("dve", lambda e: e.scalar_tensor_tensor(out=out.ap, in0=a.ap, scalar=apof(s), in1=b.ap, op0=op0, op1=op1),
                 reads=rd(a, s, b), writes=[out.t])

        def CP(ek, out, in_):
            if ek == "act":
                ACT(out, in_, AF.Copy)
            else:
                S.op(ek, lambda e: e.tensor_copy(out=out.ap, in_=in_.ap), reads=rd(in_), writes=[out.t])

        def MSET(ek, out, val):
            S.op(ek, lambda e: e.memset(out.ap, val), writes=[out.t])

        def MM(out, lhsT, rhs, start, stop, inc):
            S.op("pe", lambda e: e.matmul(out.ap, lhsT=lhsT.ap, rhs=rhs.ap, start=start, stop=stop),
                 reads=rd(lhsT, rhs), writes=[out.t], inc=inc)

        def TR(out, in_, ident, inc):
            S.op("pe", lambda e: e.transpose(out=out.ap, in_=in_.ap, identity=ident.ap), reads=rd(in_, ident), writes=[out.t], inc=inc)

        def DMA(out, in_, q="sp", **kw):
            S.dma(q, lambda e: e.dma_start(out=out.ap, in_=in_.ap, **kw), reads=[in_.t], writes=[out.t])

        P = es
        PA = Rot([T(P.enter_context(nc.psum_tensor("pa%d" % i, [128, 1024], F32))[:]) for i in range(3)])
        PTf = Rot([T(P.enter_context(nc.psum_tensor("pt%d" % i, [128, 512], F32))[:]) for i in range(2)])

        def pt_next():
            return PTf.next().v().bitcast(BF16)

        identb = sbt(P, "identb", [128, 128], BF16)
        identf = sbt(P, "identf", [128, 128], F32)
        nhalf = sbt(P, "nhalf", [128, 4], F32)
        COS = sbt(P, "COS", [128, 16, 64], F32)
        SIN = sbt(P, "SIN", [128, 16, 64], F32)
        lgf = sbt(P, "lgf", [128, 4], F32)
        lgb = sbt(P, "lgb", [128, 4], F32)
        DF = sbt(P, "DF", [128, 4], F32)
        DB = sbt(P, "DB", [128, 4], F32)
        CF = sbt(P, "CF", [128, 4], F32)
        CB = sbt(P, "CB", [128, 4], F32)
        G128F = sbt(P, "G128F", [128, 4], F32)
        G128B = sbt(P, "G128B", [128, 4], F32)
        MASKT = sbt(P, "MASKT", [128, 4, 128], F32)
        cwT = sbt(P, "cwT", [128, 4, 32], F32)
        clnT = sbt(P, "clnT", [128, 4, 2], F32)
        AFF = sbt(P, "AFF", [128, NCH, NS * NE], F32)
        WR = sbt(P, "WR", [128, 8, NE], BF16)
        st_rot = rot(P, "lnst", [128, 4, 6], F32, 8)
        mv_rot = rot(P, "lnmv", [128, 4, 2], F32, 8)
        rs_rot = rot(P, "lnrs", [128, 8], F32, 8)

        MSET("pool", nhalf.v(), -0.5)
        MSET("pool", identb.v(), 1.0)
        S.op("pool", lambda e: e.affine_select(out=identb.ap, in_=identb.ap, pattern=[[-1, 128]], compare_op=ALU.is_equal,
                                               fill=0.0, base=0, channel_multiplier=1), reads=[identb], writes=[identb])
        MSET("pool", identf.v(), 1.0)
        S.op("pool", lambda e: e.affine_select(out=identf.ap, in_=identf.ap, pattern=[[-1, 128]], compare_op=ALU.is_equal,
                                               fill=0.0, base=0, channel_multiplier=1), reads=[identf], writes=[identf])

        def ln_stats(src, F, eps=EPS):
            nch = max(1, F // 512)
            w = F // nch
            st = st_rot.next()
            for i in range(nch):
                S.op("dve", lambda e, i=i: e.bn_stats(out=st.ap[:, i, :], in_=src.ap[:, i * w:(i + 1) * w]), reads=[src.t], writes=[st])
            mv = mv_rot.next()
            S.op("dve", lambda e: e.bn_aggr(out=mv.ap[:, 0, :], in_=st.ap[:, 0:nch, :].rearrange("p a b -> p (a b)")), reads=[st], writes=[mv])
            rs = rs_rot.next()
            TS("dve", rs[:, 0:1], mv[:, 0, 1:2], eps, ALU.add)
            TT("pool", rs[:, 0:1], rs[:, 0:1], nhalf[:, 0:1], ALU.pow)
            return rs, mv

        def ln_apply(out, in_, st):
            rs, mv = st
            ACT(rs[:, 2:3], mv[:, 0, 0:1], AF.Identity, scale=rs[:, 0:1])
            ACT(rs[:, 1:2], rs[:, 2:3], AF.Identity, scale=-1.0)
            ACT(out, in_, AF.Identity, bias=rs[:, 1:2], scale=rs[:, 0:1])

        def transposeN(dst, src, n, evac="act"):
            pt = pt_next()
            for c in range(n):
                TR(pt[:, c * 128:(c + 1) * 128], src[:, c * 128:(c + 1) * 128], identb.v(), inc=(c == n - 1))
            CP(evac, dst, pt[:, 0:n * 128].re("p (c t) -> p c t", c=n))

        def pipeline(N, stages):
            cs = [dict() for _ in range(N)]
            ent = [(e if isinstance(e, tuple) else (i, e)) for i, e in enumerate(stages)]
            maxlag = max(l for l, _ in ent)
            if os.environ.get("KCHECK"):
                def tiles_of(v):
                    if isinstance(v, T):
                        yield v
                    elif isinstance(v, V):
                        yield v.t
                    elif isinstance(v, (tuple, list)):
                        for x in v:
                            yield from tiles_of(x)

                class CD(dict):
                    def __setitem__(self, k, v):
                        dict.__setitem__(self, k, v)
                        self.__dict__.setdefault("g", {})[k] = [(t, getattr(t, "gen", 0)) for t in tiles_of(v)]

                    def __getitem__(self, k):
                        for t, g in self.__dict__.get("g", {}).get(k, []):
                            assert getattr(t, "gen", 0) == g, "stale rotating buffer %r" % (k,)
                        return dict.__getitem__(self, k)

                cs = [CD() for _ in range(N)]
            for it in range(N + maxlag):
                for lag, fn in ent:
                    n = it - lag
                    if 0 <= n < N:
                        fn(n, cs[n])

        with contextlib.ExitStack() as sc:
            stg = rot(sc, "stg", [128, 8, 512], F32, 5)
            stb = rot(sc, "stb", [128, 8, 512], BF16, 3)
            jobs = []
            win_v = w_in_d.v().re("(c p) f -> p c f", p=128)
            for g in range(12):
                jobs.append((win_v[:, :, g * 512:(g + 1) * 512], WIN[:, :, g * 512:(g + 1) * 512], 8))
            for (src_d, dst) in ((w_ro_d, WRO), (w_out_d, WOUT)):
                sv = src_d.v().re("(c p) f -> p c f", p=128)
                for g in range(2):
                    jobs.append((sv[:, :, g * 512:(g + 1) * 512], dst[:, :, g * 512:(g + 1) * 512], 8))
            sv = w_co_d.v().re("(c p) f -> p c f", p=128)
            for g in range(2):
                jobs.append((sv[:, :, g * 512:(g + 1) * 512], WCO[:, :, g * 512:(g + 1) * 512], 4))
            wr32 = sbt(sc, "wr32", [128, 8, NE], F32)
            DMA(wr32.v(), w_r_d.v().re("(c p) e -> p c e", p=128))
            CP("dve", WR.v(), wr32.v())
            scT = sbt(sc, "scT", [128, 8, 5], F32)
            DMA(scT.v(), cT_d.v())
            ACT(scT.v(), scT.v(), AF.Silu)
            bada = sbt(sc, "bada", [5, 6 * D], F32)
            DMA(bada.v(), V(b_ada_d, b_ada_d.ap.partition_broadcast(5)))
            modg = rot(sc, "modg", [5, 512], F32, 3)
            g8 = sbt(sc, "g8", [8, 128], F32)
            DMA(g8.v(), gn_g_d.v().re("o (c p) -> (o c) p", p=128))
            gT = sbt(sc, "gT", [128, 8], F32)
            pa = PA.next()
            TR(pa[:, 0:8], g8.v(), identf[0:8, 0:8], inc=True)
            CP("dve", gT.v(), pa[:, 0:8])
            items = [("ada", g) for g in range(12)] + [("cast", j) for j in jobs]

            def su_load(i, c):
                kind, arg = items[i]
                a = stg.next()
                if kind == "ada":
                    DMA(a.v(), w_ada_d.v().re("(c p) f -> p c f", p=128)[:, :, arg * 512:(arg + 1) * 512])
                else:
                    DMA(a[:, 0:arg[2], :], arg[0])
                c["a"] = a

            def su_comp(i, c):
                kind, arg = items[i]
                a = c["a"]
                if kind == "ada":
                    g = arg
                    pa = PA.next()
                    for kc in range(8):
                        MM(pa[0:5, 0:512], scT[:, kc, :], a[:, kc, :], start=(kc == 0), stop=(kc == 7), inc=(kc == 7))
                    mg = modg.next()
                    if g in (2, 3, 8, 9):
                        STT(mg.v(), pa[0:5, 0:512], 1.0, bada[:, g * 512:(g + 1) * 512], ALU.add, ALU.add)
                    else:
                        TT("dve", mg.v(), pa[0:5, 0:512], bada[:, g * 512:(g + 1) * 512], ALU.add)
                    c["o"] = mg
                else:
                    nc_ = arg[2]
                    b_ = stb.next()
                    if arg[1].t is WRO:
                        TT("dve", a[:, 0:nc_, :], a[:, 0:nc_, :], gT.v().un(2).bc([128, 8, 512]), ALU.mult)
                    CP("dve" if i % 2 == 0 else "act", b_[:, 0:nc_, :], a[:, 0:nc_, :])
                    c["o"] = b_

            def su_store(i, c):
                kind, arg = items[i]
                if kind == "ada":
                    DMA(MOD[:, arg * 512:(arg + 1) * 512], c["o"].v())
                else:
                    DMA(arg[1], c["o"][:, 0:arg[2], :])

            pipeline(len(items), [(0, su_load), (3, su_comp), (4, su_store)])
            pcol_i = sbt(sc, "pcol_i", [128, 1], I32)
            pcol = sbt(sc, "pcol", [128, 1], F32)
            S.op("pool", lambda e: e.iota(pcol_i.ap, pattern=[[0, 1]], base=0, channel_multiplier=1), writes=[pcol_i])
            CP("dve", pcol.v(), pcol_i.v())
            hi = sbt(sc, "hi", [128, 1], F32)
            TS("dve", hi.v(), pcol.v(), 64.0, ALU.is_ge)
            colp = sbt(sc, "colp", [128, 1], F32)
            STT(colp.v(), hi.v(), -64.0, pcol.v(), ALU.mult, ALU.add)
            rown_i = sbt(sc, "rown_i", [128, 16], I32)
            rown = sbt(sc, "rown", [128, 16], F32)
            S.op("pool", lambda e: e.iota(rown_i.ap, pattern=[[2, 16]], base=0, channel_multiplier=0), writes=[rown_i])
            CP("dve", rown.v(), rown_i.v())
            TS("dve", rown.v(), rown.v(), hi.v(), ALU.add)
            fr_i = sbt(sc, "fr_i", [128, 32], I32)
            fr = sbt(sc, "fr", [128, 32], F32)
            S.op("pool", lambda e: e.iota(fr_i.ap, pattern=[[1, 32]], base=0, channel_multiplier=0), writes=[fr_i])
            CP("dve", fr.v(), fr_i.v())
            ACT(fr.v(), fr.v(), AF.Exp, scale=-math.log(10000.0) / 32.0)
            ang = sbt(sc, "ang", [128, 16, 64], F32)
            for n in range(16):
                TS("dve", ang[:, n, 0:32], fr.v(), rown[:, n:n + 1], ALU.mult)
                TS("dve", ang[:, n, 32:64], fr.v(), colp.v(), ALU.mult)
            yq = sbt(sc, "yq", [128, 1024], F32)
            yi = sbt(sc, "yi", [128, 1024], I32)
            yf = sbt(sc, "yf", [128, 1024], F32)
            ngm = sbt(sc, "ngm", [128, 1024], F32)
            angf = ang.v().re("p n i -> p (n i)")
            for (dst, shift) in ((SIN, 0.0), (COS, math.pi / 2.0)):
                TS("dve", yq.v(), angf, 1.0 / (2 * math.pi), ALU.mult, 0.5 + shift / (2 * math.pi), ALU.add)
                CP("dve", yi.v(), yq.v())
                CP("dve", yf.v(), yi.v())
                TT("dve", yq.v(), yq.v(), yf.v(), ALU.subtract)
                TS("dve", ngm.v(), yq.v(), 0.0, ALU.is_lt)
                TT("dve", yq.v(), yq.v(), ngm.v(), ALU.add)
                TS("dve", yq.v(), yq.v(), -0.5, ALU.add, 2 * math.pi, ALU.mult)
                TS("dve", yq.v(), yq.v(), -math.pi, ALU.max, math.pi, ALU.min)
                ACT(dst.v().re("p n i -> p (n i)"), yq.v(), AF.Sin)
            DMA(lgf.v(), V(lgf_d, lgf_d.ap.partition_broadcast(128)))
            DMA(lgb.v(), V(lgb_d, lgb_d.ap.partition_broadcast(128)))
            tmpc = sbt(sc, "tmpc", [128, 1], F32)
            tmp4 = sbt(sc, "tmp4", [128, 4], F32)

            def dec_table(dst, lg, mul, add, post):
                TS("dve", tmpc.v(), pcol.v(), float(mul), ALU.mult, float(add), ALU.add)
                TS("dve", tmp4.v(), lg.v(), tmpc.v(), ALU.mult)
                ACT(tmp4.v(), tmp4.v(), AF.Exp)
                TS("dve", dst.v(), tmp4.v(), float(post), ALU.mult)

            dec_table(DF, lgf, -1.0, 127.0, DKS)
            dec_table(DB, lgb, 1.0, 0.0, DKS)
            dec_table(CF, lgf, 1.0, 1.0, 1.0)
            dec_table(CB, lgb, -1.0, 128.0, 1.0)
            dec_table(G128F, lgf, 0.0, 128.0, 1.0)
            dec_table(G128B, lgb, 0.0, 128.0, 1.0)
            dif_i = sbt(sc, "dif_i", [128, 128], I32)
            dpos = sbt(sc, "dpos", [128, 128], F32)
            dneg = sbt(sc, "dneg", [128, 128], F32)
            S.op("pool", lambda e: e.iota(dif_i.ap, pattern=[[1, 128]], base=0, channel_multiplier=-1), writes=[dif_i])
            CP("dve", dpos.v(), dif_i.v())
            TS("dve", dneg.v(), dpos.v(), -1.0, ALU.mult, 0.0, ALU.max)
            TS("dve", dpos.v(), dpos.v(), 0.0, ALU.max)
            mtmp = sbt(sc, "mtmp", [128, 128], F32)
            for h in range(4):
                TS("dve", mtmp.v(), dpos.v(), lgf[:, h:h + 1], ALU.mult)
                STT(mtmp.v(), dneg.v(), lgb[:, h:h + 1], mtmp.v(), ALU.mult, ALU.add)
                ACT(mtmp.v(), mtmp.v(), AF.Exp)
                TS("dve", MASKT[:, h, :], mtmp.v(), DKS, ALU.mult)
            cw = sbt(sc, "cw", [32, 512], F32)
            DMA(cw.v(), convw_d.v())
            pa = PA.next()
            for cc in range(4):
                TR(pa[:, cc * 32:(cc + 1) * 32], cw[:, cc * 128:(cc + 1) * 128], identf[0:32, 0:32], inc=(cc == 3))
            CP("dve", cwT.v(), pa[:, 0:128].re("p (c k) -> p c k", c=4))
            cl2 = sbt(sc, "cl2", [2, 512], F32)
            DMA(cl2[0:1, :], cln_g_d.v())
            DMA(cl2[1:2, :], cln_b_d.v())
            pa = PA.next()
            for cc in range(4):
                TR(pa[:, cc * 2:(cc + 1) * 2], cl2[:, cc * 128:(cc + 1) * 128], identf[0:2, 0:2], inc=(cc == 3))
            CP("dve", clnT.v(), pa[:, 0:8].re("p (c k) -> p c k", c=4))
            S.barrier()

        def mm_wide(ps, lhs_fn, w, nk):
            for hf in range(2):
                for kc in range(nk):
                    MM(ps[:, hf * 512:(hf + 1) * 512], lhs_fn(kc), w[:, kc, hf * 512:(hf + 1) * 512],
                       start=(kc == 0), stop=(kc == nk - 1), inc=(kc == nk - 1))

        for b in range(NS if "KSTOP" not in os.environ else 1):
            with contextlib.ExitStack() as sA:
                hT_all = sbt(sA, "hT", [128, 8, 18 * 128], BF16)
                hT = [T(hT_all.ap[:, :, j * 128:(j + 1) * 128]) for j in range(18)]
                VS_all = sbt(sA, "VS", [128, NCH, 1024], BF16)
                VS = [T(VS_all.ap[:, n, :]) for n in range(NCH)]

                def load_bc(sc, name, src_t, row, c0, n=1024):
                    t = sbt(sc, name, [128, n], F32)
                    DMA(t.v(), V(src_t, src_t.ap[row:row + 1, c0:c0 + n].partition_broadcast(128)))
                    return t

                with contextlib.ExitStack() as sc:
                    SH1 = load_bc(sc, "SH1", MOD, b, 0)
                    SC1 = load_bc(sc, "SC1", MOD, b, 1024)
                    SH1c = load_bc(sc, "SH1c", MOD, 4, 0)
                    SC1c = load_bc(sc, "SC1c", MOD, 4, 1024)
                    xin = rot(sc, "xin", [128, 1024], F32, 5)
                    xn = rot(sc, "xn", [128, 1024], F32, 2)
                    hb = rot(sc, "hb", [128, 1024], BF16, 3)

                    def s0_l(j, c):
                        xi = xin.next()
                        if j < 2:
                            DMA(xi.v(), ctx_d[b, j * 128:(j + 1) * 128, :])
                        else:
                            DMA(xi.v(), x_d[b, (j - 2) * 128:(j - 1) * 128, :])
                        c["xi"] = xi

                    def s0_l2(j, c):
                        pass

                    def s0_a(j, c):
                        c["rs"] = ln_stats(c["xi"].v(), 1024)

                    def s0_b(j, c):
                        sc_t, sh_t = (SC1c, SH1c) if j < 2 else (SC1, SH1)
                        xnt = xn.next()
                        ln_apply(xnt.v(), c["xi"].v(), c["rs"])
                        TT("dve", xnt.v(), xnt.v(), sc_t.v(), ALU.mult)
                        hbt = hb.next()
                        TT("pool", hbt.v(), xnt.v(), sh_t.v(), ALU.add)
                        c["hb"] = hbt

                    def s0_c(j, c):
                        transposeN(hT[j].v(), c["hb"].v(), 8)

                    pipeline(18, [s0_l, s0_l2, s0_a, s0_b, s0_c])
                    S.barrier()
                    _chk("s0")

                with contextlib.ExitStack() as sR:
                    kT_all = sbt(sR, "kT", [128, 4, L], BF16)
                    kT = [T(kT_all.ap[:, :, n * 128:(n + 1) * 128]) for n in range(NCH)]
                    KR_all = sbt(sR, "KR", [128, NCH, 512], BF16)
                    KR = [T(KR_all.ap[:, n, :].rearrange("p (h d) -> p h d", h=4)) for n in range(NCH)]
                    SFrun = sbt(sR, "SFrun", [128, 1024], F32)
                    SB_all = sbt(sR, "SB", [128, NCH, 1024], BF16)
                    SB = [T(SB_all.ap[:, n, :]) for n in range(NCH)]

                    def rope(ps, dst, n, tmp_rot):
                        pv = ps.re("p (h i two) -> p h i two", h=4, two=2)
                        t1, t2 = pv[:, :, :, 0], pv[:, :, :, 1]
                        if n is None:
                            CP("act", dst[:, :, 0:64], t1)
                            CP("act", dst[:, :, 64:128], t2)
                            return
                        cs = COS[:, n, :].un(1).bc([128, 4, 64])
                        sn = SIN[:, n, :].un(1).bc([128, 4, 64])
                        a = tmp_rot.next()
                        b2 = tmp_rot.next()
                        TT("dve", a.v(), t1, cs, ALU.mult)
                        TT("dve", b2.v(), t2, sn, ALU.mult)
                        TT("pool", dst[:, :, 0:64], a.v(), b2.v(), ALU.subtract)
                        c2 = tmp_rot.next()
                        d2 = tmp_rot.next()
                        TT("dve", c2.v(), t1, sn, ALU.mult)
                        TT("dve", d2.v(), t2, cs, ALU.mult)
                        TT("pool", dst[:, :, 64:128], c2.v(), d2.v(), ALU.add)

                    with contextlib.ExitStack() as sc:
                        Wkv = sbt(sc, "Wkv", [128, 8, 1536], BF16)
                        DMA(Wkv.v(), WIN[:, :, 1536:3072])
                        rtmp = rot(sc, "rtmp", [128, 4, 64], F32, 4)
                        krot = rot(sc, "krot", [128, 4, 128], BF16, 2)
                        kf = rot(sc, "kf", [128, 4, 128], BF16, 2)
                        kb = rot(sc, "kb", [128, 4, 128], BF16, 2)
                        vtmp = rot(sc, "vtmp", [128, 1024], BF16, 2)
                        SBrun = sbt(sc, "SBrun", [128, 1024], F32)

                        def s1_a(j, c):
                            n = j - 2
                            pk = PA.next()
                            pv_ = PA.next()
                            for kc in range(8):
                                MM(pk[:, 0:512], hT[j][:, kc, :], Wkv[:, kc, 0:512], start=(kc == 0), stop=(kc == 7), inc=(kc == 7))
                            for hf in range(2):
                                for kc in range(8):
                                    MM(pv_[:, hf * 512:(hf + 1) * 512], hT[j][:, kc, :], Wkv[:, kc, 512 + hf * 512:1024 + hf * 512],
                                       start=(kc == 0), stop=(kc == 7), inc=(kc == 7))
                            vt = VS[n] if n >= 0 else vtmp.next()
                            CP("act", vt.v(), pv_.v())
                            kr = KR[n] if n >= 0 else krot.next()
                            rope(pk[:, 0:512], kr, n if n >= 0 else None, rtmp)
                            c["vt"], c["kr"] = vt, kr
                            if n < 0:
                                c["kft"] = kf.next()
                                TT("pool", c["kft"].v(), kr.v(), DF.v().un(2).bc([128, 4, 128]), ALU.mult)
                            if n != 0:
                                c["kbt"] = kb.next()
                                TT("pool", c["kbt"].v(), kr.v(), DB.v().un(2).bc([128, 4, 128]), ALU.mult)

                        def s1_b(j, c):
                            n = j - 2
                            vt, kr = c["vt"], c["kr"]
                            if n >= 0:
                                pt = pt_next()
                                for h in range(4):
                                    TR(pt[:, h * 128:(h + 1) * 128], kr[:, h, :], identb.v(), inc=(h == 3))
                                CP("act", kT[n].v(), pt[:, 0:512].re("p (h t) -> p h t", h=4))
                            if n < 0:
                                kft = c["kft"]
                                pf = PA.next()
                                for h in range(4):
                                    MM(pf[:, h * 256:(h + 1) * 256], kft[:, h, :], vt[:, h * 256:(h + 1) * 256], start=True, stop=True, inc=(h == 3))
                                if j == 0:
                                    CP("act", SFrun.v(), pf.v())
                                else:
                                    for h in range(4):
                                        sl = slice(h * 256, (h + 1) * 256)
                                        STT(SFrun[:, sl], SFrun[:, sl], G128F[:, h:h + 1], pf[:, sl], ALU.mult, ALU.add)
                            if n != 0:
                                kbt = c["kbt"]
                                pb = PA.next()
                                for h in range(4):
                                    MM(pb[:, h * 256:(h + 1) * 256], kbt[:, h, :], vt[:, h * 256:(h + 1) * 256], start=True, stop=True, inc=(h == 3))
                                if j == 0:
                                    CP("act", SBrun.v(), pb.v())
                                elif j == 1:
                                    for h in range(4):
                                        sl = slice(h * 256, (h + 1) * 256)
                                        STT(SBrun[:, sl], pb[:, sl], G128B[:, h:h + 1], SBrun[:, sl], ALU.mult, ALU.add)
                                    CP("act", SB[15].v(), SBrun.v())
                                else:
                                    CP("act", SB[n - 1].v(), pb.v())

                        pipeline(18, [s1_a, s1_b])
                        for n in range(15, 0, -1):
                            for h in range(4):
                                sl = slice(h * 256, (h + 1) * 256)
                                STT(SBrun[:, sl], SBrun[:, sl], G128B[:, h:h + 1], SB[n - 1][:, sl], ALU.mult, ALU.add)
                            CP("act", SB[n - 1].v(), SBrun.v())
                        S.barrier()
                        _chk("s1")

                    with contextlib.ExitStack() as sc:
                        Wq = sbt(sc, "Wq", [128, 8, 512], BF16)
                        DMA(Wq.v(), WIN[:, :, 1024:1536])
                        rtmp = rot(sc, "rtmp2", [128, 4, 64], F32, 4)
                        kf = rot(sc, "kf2", [128, 4, 128], BF16, 4)
                        SFb = rot(sc, "SFb", [128, 1024], BF16, 2)
                        qrot = rot(sc, "qrot", [128, 4, 128], BF16, 2)
                        CFT = sbt(sc, "CFT", [128, 4, 128], F32)
                        CBT = sbt(sc, "CBT", [128, 4, 128], F32)
                        dti = sbt(sc, "dti", [128, 4, 128], I32)
                        for (tab, lg, step, base) in ((CFT, lgf, 1, 1), (CBT, lgb, -1, 128)):
                            S.op("pool", lambda e, step=step, base=base: e.iota(dti.ap, pattern=[[0, 4], [step, 128]], base=base, channel_multiplier=0),
                                 writes=[dti])
                            CP("dve", tab.v(), dti.v())
                            TT("dve", tab.v(), tab.v(), lg.v().un(2).bc([128, 4, 128]), ALU.mult)
                            ACT(tab.v(), tab.v(), AF.Exp)
                        qT3 = rot(sc, "qT3", [128, 3, 4, 128], BF16, 3)
                        Pm = rot(sc, "Pm", [128, 4, 128], BF16, 3)

                        porot = Rot([PA.ts[1], PA.ts[2]])
                        pfrot = Rot([PA.ts[0]])

                        def s2a_a(n, c):
                            j = n + 2
                            pq = PTf.next()
                            for kc in range(8):
                                MM(pq[:, 0:512], hT[j][:, kc, :], Wq[:, kc, :], start=(kc == 0), stop=(kc == 7), inc=(kc == 7))
                            qr = qrot.next()
                            rope(pq[:, 0:512], qr, n, rtmp)
                            c["qr"] = qr
                            if n < NCH - 1:
                                c["kft"] = kf.next()
                                TT("pool", c["kft"].v(), KR[n].v(), DF.v().un(2).bc([128, 4, 128]), ALU.mult)

                        def s2a_b(n, c):
                            qr = c["qr"]
                            qt = qT3.next()
                            pt = pt_next()
                            for h in range(4):
                                TR(pt[:, h * 128:(h + 1) * 128], qr[:, h, :], identb.v(), inc=(h == 3))
                            CP("act", qt[:, 0, :, :], pt[:, 0:512].re("p (h t) -> p h t", h=4))
                            TT("pool", qt[:, 1, :, :], qt[:, 0, :, :], CFT.v(), ALU.mult)
                            TT("pool", qt[:, 2, :, :], qt[:, 0, :, :], CBT.v(), ALU.mult)
                            c["qt"] = qt

                        def s2a_c(n, c):
                            qt = c["qt"]
                            pa_ = PTf.next()
                            for h in range(4):
                                MM(pa_[:, h * 128:(h + 1) * 128], kT[n][:, h, :], qt[:, 0, h, :], start=True, stop=True, inc=(h == 3))
                            pm = Pm.next()
                            TT("dve", pm.v(), pa_[:, 0:512].re("p (h t) -> p h t", h=4), MASKT.v(), ALU.mult)
                            c["pm"] = pm

                        sfb_cur = [SFb.next()]
                        CP("act", sfb_cur[0].v(), SFrun.v())

                        def s2a_d(n, c):
                            qt, pm = c["qt"], c["pm"]
                            sfb = sfb_cur[0]
                            po = porot.next()
                            for h in range(4):
                                sl = slice(h * 256, (h + 1) * 256)
                                MM(po[:, sl], pm[:, h, :], VS[n][:, sl], start=True, stop=False, inc=False)
                                MM(po[:, sl], qt[:, 1, h, :], sfb[:, sl], start=False, stop=False, inc=False)
                                MM(po[:, sl], qt[:, 2, h, :], SB[n][:, sl], start=False, stop=True, inc=(h == 3))
                            if n < NCH - 1:
                                kft = c["kft"]
                                pf = pfrot.next()
                                for h in range(4):
                                    MM(pf[:, h * 256:(h + 1) * 256], kft[:, h, :], VS[n][:, h * 256:(h + 1) * 256], start=True, stop=True, inc=(h == 3))
                                for h in range(4):
                                    sl = slice(h * 256, (h + 1) * 256)
                                    STT(SFrun[:, sl], SFrun[:, sl], G128F[:, h:h + 1], pf[:, sl], ALU.mult, ALU.add)
                            st = st_rot.next()
                            mv = mv_rot.next()
                            for h in range(4):
                                S.op("dve", lambda e, h=h: e.bn_stats(out=st.ap[:, h, :], in_=po.ap[:, h * 256:(h + 1) * 256]), reads=[po], writes=[st])
                            for h in range(4):
                                S.op("dve", lambda e, h=h: e.bn_aggr(out=mv.ap[:, h, :], in_=st.ap[:, h, :]), reads=[st], writes=[mv])
                            rs = rs_rot.next()
                            TS("dve", rs[:, 0:4], mv[:, :, 1], EPS, ALU.add)
                            TT("pool", rs[:, 0:4], rs[:, 0:4], nhalf.v(), ALU.pow)
                            TT("pool", rs[:, 4:8], mv[:, :, 0], rs[:, 0:4], ALU.mult)
                            TS("pool", rs[:, 4:8], rs[:, 4:8], -1.0, ALU.mult, 1.0, ALU.mult)
                            c["po"], c["rs"] = po, rs

                        def s2a_d2(n, c):
                            if n < NCH - 1:
                                sfb_cur[0] = SFb.next()
                                CP("act", sfb_cur[0].v(), SFrun.v())

                        def s2a_e(n, c):
                            po, rs = c["po"], c["rs"]
                            for h in range(4):
                                sl = slice(h * 256, (h + 1) * 256)
                                ACT(VS[n][:, sl], po[:, sl], AF.Identity, bias=rs[:, 4 + h:5 + h], scale=rs[:, h:h + 1])

                        pipeline(NCH, [(0, s2a_a), (1, s2a_b), (2, s2a_c), (3, s2a_d), (4, s2a_e), (3, s2a_d2)])
                        S.barrier()
                        _chk("s2a")

                with contextlib.ExitStack() as sc:
                    Wgr = sbt(sc, "Wgr", [128, 8, 1024], BF16)
                    Wgb = sbt(sc, "Wgb", [128, 8, 1024], BF16)
                    Wro = sbt(sc, "Wro", [128, 8, 1024], BF16)
                    DMA(Wgr.v(), WIN[:, :, 3072:4096])
                    DMA(Wgb.v(), WIN[:, :, 5120:6144])
                    DMA(Wro.v(), WRO.v())
                    sg = rot(sc, "sg", [128, 1024], F32, 2)
                    ybin = rot(sc, "ybin", [128, 1024], BF16, 3)
                    ybT = rot(sc, "ybT", [128, 8, 128], BF16, 3)
                    sgb = rot(sc, "sgb", [128, 1024], F32, 4)

                    def s2b_a(n, c):
                        j = n + 2
                        pg = PA.next()
                        mm_wide(pg, lambda kc: hT[j][:, kc, :], Wgr, 8)
                        s1 = sg.next()
                        ACT(s1.v(), pg.v(), AF.Silu)
                        yb = ybin.next()
                        TT("pool", yb.v(), s1.v(), VS[n].v(), ALU.mult)
                        pgb = PA.next()
                        mm_wide(pgb, lambda kc: hT[j][:, kc, :], Wgb, 8)
                        s2 = sgb.next()
                        ACT(s2.v(), pgb.v(), AF.Sigmoid)
                        c["yb"], c["s2"] = yb, s2

                    def s2b_b(n, c):
                        ybt = ybT.next()
                        transposeN(ybt.v(), c["yb"].v(), 8)
                        c["ybt"] = ybt

                    def s2b_c(n, c):
                        ybt = c["ybt"]
                        py = PA.next()
                        mm_wide(py, lambda kc: ybt[:, kc, :], Wro, 8)
                        TT("dve", VS[n].v(), py.v(), c["s2"].v(), ALU.mult)

                    pipeline(NCH, [s2b_a, s2b_b, s2b_c])
                    S.barrier()
                    _chk("s2b")

                with contextlib.ExitStack() as sCv:
                    cT_all = sbt(sCv, "cT", [128, 4, L], BF16)
                    cT = [T(cT_all.ap[:, :, n * 128:(n + 1) * 128]) for n in range(NCH)]
                    with contextlib.ExitStack() as sY:
                        yT = sbt(sY, "yT", [128, 4, L + 30], BF16)
                        DGa = sbt(sY, "DG", [128, 4, 31, 128], BF16)
                        DG = [T(DGa.ap[:, cc, :, :]) for cc in range(4)]
                        MSET("pool", yT[:, :, 0:15], 0.0)
                        MSET("pool", yT[:, :, L + 15:L + 30], 0.0)
                        for cc in range(4):
                            for k in range(31):
                                TS("pool" if cc % 2 else "dve", DG[cc][:, k, :], identf.v(), cwT[:, cc, k:k + 1], ALU.mult, 1.0, ALU.mult)
                        with contextlib.ExitStack() as sc:
                            Wu = sbt(sc, "Wu", [128, 8, 1024], BF16)
                            DMA(Wu.v(), WIN[:, :, 0:1024])
                            sgu = rot(sc, "sgu", [128, 512], F32, 2)
                            yg = rot(sc, "yg", [128, 512], BF16, 3)

                            def c1_a(n, c):
                                j = n + 2
                                pu = PA.next()
                                mm_wide(pu, lambda kc: hT[j][:, kc, :], Wu, 8)
                                s1 = sgu.next()
                                ACT(s1.v(), pu[:, 512:1024], AF.Sigmoid)
                                y1 = yg.next()
                                TT("dve", y1.v(), pu[:, 0:512], s1.v(), ALU.mult)
                                c["y1"] = y1

                            def c1_b(n, c):
                                y1 = c["y1"]
                                pt = pt_next()
                                for cc in range(4):
                                    TR(pt[:, cc * 128:(cc + 1) * 128], y1[:, cc * 128:(cc + 1) * 128], identb.v(), inc=(cc == 3))
                                CP("act", yT[:, :, 15 + n * 128:15 + (n + 1) * 128], pt[:, 0:512].re("p (c t) -> p c t", c=4))

                            pipeline(NCH, [c1_a, c1_b])
                            S.barrier()
                            _chk("c1")
                        with contextlib.ExitStack() as sc:
                            cf = rot(sc, "cf", [128, 4, 512], F32, 2)
                            cs_ = rot(sc, "cs", [128, 512], BF16, 3)
                            cfts = {}

                            pcrot = Rot([T(PA.ts[0].ap[:, 0:512]), T(PA.ts[0].ap[:, 512:1024])])
                            plrot = Rot([T(PA.ts[1].ap[:, 0:512]), T(PA.ts[1].ap[:, 512:1024]),
                                         T(PA.ts[2].ap[:, 0:512]), T(PA.ts[2].ap[:, 512:1024])])

                            def c2_a(n, c):
                                tg, cc = n // 4, n % 4
                                if cc == 0:
                                    cfts[tg] = cf.next()
                                cft = cfts[tg]
                                pc = pcrot.next()
                                for k in range(31):
                                    MM(pc.v(), DG[cc][:, k, :], yT[:, cc, tg * 512 + k:tg * 512 + k + 512],
                                       start=(k == 0), stop=(k == 30), inc=(k == 30))
                                ACT(cft[:, cc, :], pc.v(), AF.Identity, bias=cwT[:, cc, 31:32])

                            def c2_b(n, c):
                                cft = cfts[n // 4]
                                i = n % 4
                                pl = plrot.next()
                                for cc in range(4):
                                    TR(pl[:, cc * 128:(cc + 1) * 128], cft[:, cc, i * 128:(i + 1) * 128], identf.v(), inc=(cc == 3))
                                c["rs"] = ln_stats(pl.v(), 512)
                                c["pl"] = pl

                            def c2_c(n, c):
                                c2 = cs_.next()
                                ln_apply(c2.v(), c["pl"].v(), c["rs"])
                                c["c2"] = c2

                            def c2_d(n, c):
                                c2 = c["c2"]
                                pt = pt_next()
                                for cc in range(4):
                                    TR(pt[:, cc * 128:(cc + 1) * 128], c2[:, cc * 128:(cc + 1) * 128], identb.v(), inc=(cc == 3))
                                for cc in range(4):
                                    ACT(cT[n][:, cc, :], pt[:, cc * 128:(cc + 1) * 128], AF.Silu, bias=clnT[:, cc, 1:2], scale=clnT[:, cc, 0:1])

                            pipeline(NCH, [(0, c2_a), (4, c2_b), (5, c2_c), (6, c2_d)])
                            S.barrier()
                            _chk("c2")
                    with contextlib.ExitStack() as sc:
                        Wga = sbt(sc, "Wga", [128, 8, 1024], BF16)
                        Wco = sbt(sc, "Wco", [128, 4, 1024], BF16)
                        Wout = sbt(sc, "Wout", [128, 8, 1024], BF16)
                        DMA(Wco.v(), WCO.v())
                        DMA(Wga.v(), WIN[:, :, 4096:5120])
                        DMA(Wout.v(), WOUT.v())
                        L1G = load_bc(sc, "L1G", ln1_g_d, 0, 0)
                        L1B = load_bc(sc, "L1B", ln1_b_d, 0, 0)
                        SH2 = load_bc(sc, "SH2", MOD, b, 3072)
                        SC2 = load_bc(sc, "SC2", MOD, b, 4096)
                        sga = rot(sc, "sga", [128, 1024], F32, 1)
                        G1 = sga.ts[0]
                        DMA(G1.v(), V(MOD, MOD.ap[b:b + 1, 2048:3072].partition_broadcast(128)))
                        for kc in range(8):
                            TT("pool", Wout[:, kc, :], Wout[:, kc, :], G1.v(), ALU.mult)
                        mb = rot(sc, "mb", [128, 1024], BF16, 2)
                        mT = rot(sc, "mT", [128, 8, 128], BF16, 2)
                        xin = rot(sc, "xin3", [128, 1024], F32, 3)
                        rr = rot(sc, "rr", [128, 1024], F32, 4)
                        h2b = rot(sc, "h2b", [128, 1024], BF16, 2)
                        h2T = rot(sc, "h2T", [128, 8, 128], BF16, 2)
                        sm = rot(sc, "sm", [128, 20], F32, 3)

                        def p3_a(n, c):
                            j = n + 2
                            xi = xin.next()
                            DMA(xi.v(), x_d[b, n * 128:(n + 1) * 128, :])
                            pya = PA.next()
                            mm_wide(pya, lambda kc: cT[n][:, kc, :], Wco, 4)
                            pga = PA.next()
                            mm_wide(pga, lambda kc: hT[j][:, kc, :], Wga, 8)
                            s1 = sga.next()
                            ACT(s1.v(), pga.v(), AF.Sigmoid)
                            c["xi"], c["s1"], c["pya"] = xi, s1, pya

                        def p3_a2(n, c):
                            s1 = c["s1"]
                            TT("dve", s1.v(), c["pya"].v(), s1.v(), ALU.mult)
                            m2 = mb.next()
                            TT("pool", m2.v(), s1.v(), VS[n].v(), ALU.add)
                            c["m2"] = m2

                        def p3_b(n, c):
                            mt = mT.next()
                            transposeN(mt.v(), c["m2"].v(), 8)
                            c["mt"] = mt

                        def p3_c(n, c):
                            mt = c["mt"]
                            pyo = PA.next()
                            mm_wide(pyo, lambda kc: mt[:, kc, :], Wout, 8)
                            c["pyo"] = pyo

                        def p3_c2(n, c):
                            pyo = c["pyo"]
                            r1 = rr.next()
                            STT(r1.v(), c["xi"].v(), ALPHA, pyo.v(), ALU.mult, ALU.add)
                            c["rs1"] = ln_stats(r1.v(), 1024)
                            c["xo"] = r1

                        def p3_d(n, c):
                            xo = c["xo"]
                            ln_apply(xo.v(), xo.v(), c["rs1"])
                            TT("dve", xo.v(), xo.v(), L1G.v(), ALU.mult)
                            TT("pool", xo.v(), xo.v(), L1B.v(), ALU.add)
                            DMA(ACC[b][n * 128:(n + 1) * 128, :], xo.v())
                            if debug:
                                DMA(DBG_X1[b * L + n * 128:b * L + (n + 1) * 128, :], xo.v())

                        def p3_d2(n, c):
                            c["rs2"] = ln_stats(c["xo"].v(), 1024)

                        def p3_e(n, c):
                            xo = c["xo"]
                            ln_apply(xo.v(), xo.v(), c["rs2"])
                            TT("dve", xo.v(), xo.v(), SC2.v(), ALU.mult)
                            hb_ = h2b.next()
                            TT("pool", hb_.v(), xo.v(), SH2.v(), ALU.add)
                            DMA(H2[b * L + n * 128:b * L + (n + 1) * 128, :], hb_.v())
                            c["hb"] = hb_

                        def p3_f(n, c):
                            ht = h2T.next()
                            transposeN(ht.v(), c["hb"].v(), 8)
                            c["ht"] = ht

                        def p3_g(n, c):
                            ht = c["ht"]
                            pr = PTf.next()
                            for kc in range(8):
                                MM(pr[:, 0:NE], ht[:, kc, :], WR[:, kc, :], start=(kc == 0), stop=(kc == 7), inc=(kc == 7))
                            s_ = sm.next()
                            S.op("dve", lambda e, s_=s_, pr=pr: e.tensor_reduce(out=s_.ap[:, 16:17], in_=pr.ap[:, 0:NE], axis=mybir.AxisListType.X, op=ALU.max),
                                 reads=[pr], writes=[s_])
                            TS("dve", s_[:, 17:18], s_[:, 16:17], -1.0, ALU.mult)
                            ACT(s_[:, 0:16], pr[:, 0:NE], AF.Exp, bias=s_[:, 17:18])
                            S.op("dve", lambda e, s_=s_: e.tensor_reduce(out=s_.ap[:, 18:19], in_=s_.ap[:, 0:16], axis=mybir.AxisListType.X, op=ALU.add),
                                 reads=[s_], writes=[s_])
                            S.op("dve", lambda e, s_=s_: e.reciprocal(out=s_.ap[:, 19:20], in_=s_.ap[:, 18:19]), reads=[s_], writes=[s_])
                            TS("dve", AFF[:, n, b * NE:(b + 1) * NE], s_[:, 0:16], s_[:, 19:20], ALU.mult)

                        pipeline(NCH, [(4, p3_d2), (5, p3_e), (3, p3_d), (7, p3_g), (6, p3_f), (0, p3_a), (1, p3_b), (2, p3_c), (0, p3_a2), (2, p3_c2)])
                        S.barrier()
                        _chk("p3")

        IDXT = sbt(P, "IDXT", [128, 2, NS * NE], I32)
        GT = sbt(P, "GT", [128, 2, NS * NE], F32)
        with contextlib.ExitStack() as sc:
            G2 = [sbt(sc, "G2_%d" % b, [128, 1024], F32) for b in range(NS)]
            for b in range(NS):
                DMA(G2[b].v(), V(MOD, MOD.ap[b:b + 1, 5120:6144].partition_broadcast(128)))
                TS("pool", G2[b].v(), G2[b].v(), 1.0 / ALPHA, ALU.mult, 1.0, ALU.mult)
            wbf = [[sbt(sc, "wbf%d_%d" % (m, i), [128, 8, 1024], BF16) for i in range(2)] for m in range(3)]
            wbfp = [[[T(wbf[m][i].ap[:, 2 * q:2 * q + 2, :]) for q in range(4)] for i in range(2)] for m in range(3)]
            wst = rot(sc, "wst", [128, 2, 1024], F32, 2)
            wsrc = (w_gate_d, w_up_d, w_down_d)
            castn = [0]
            cast_engs = ["pool", "act"]

            def weight_pieces(e_):
                buf = e_ % 2
                fns = []
                for m in range(3):
                    wv = wsrc[m].v()[e_].re("(c p) f -> p c f", p=128)
                    for q in range(4):
                        def f(m=m, q=q, wv=wv):
                            st_ = wst.next()
                            DMA(st_.v(), wv[:, 2 * q:2 * q + 2, :])
                            CP(cast_engs[castn[0] % len(cast_engs)], wbfp[m][buf][q].v(), st_.v())
                            castn[0] += 1
                        fns.append(f)
                return fns

            def gather(e_, g):
                b, half = g // 2, g % 2
                xg = xe.next()
                col = b * NE + e_
                S.dma("pool", lambda e, xg=xg, half=half, col=col: e.indirect_dma_start(
                    out=xg.ap, out_offset=None, in_=H2.ap,
                    in_offset=bass.IndirectOffsetOnAxis(ap=IDXT.ap[:, half, col:col + 1], axis=0)),
                    reads=[H2, IDXT], writes=[xg])
                return xg

            for f in weight_pieces(0):
                f()
            cast_engs[:] = ["act", "act", "dve"]
            with contextlib.ExitStack() as scB:
                AT = sbt(scB, "AT", [64, L], F32)
                vals = sbt(scB, "vals", [64, CAP], F32)
                idxu = sbt(scB, "idxu", [64, CAP], U32)
                idxf = sbt(scB, "idxf", [64, CAP], F32)
                for n0 in range(0, NCH, 4):
                    pa = PA.next()
                    for i in range(4):
                        TR(pa[0:64, i * 128:(i + 1) * 128], AFF[:, n0 + i, :], identf.v(), inc=(i == 3))
                    CP("act", AT[:, n0 * 128:(n0 + 4) * 128], pa[0:64, 0:512])
                for it in range(CAP // 8):
                    sl = slice(it * 8, (it + 1) * 8)
                    S.op("dve", lambda e, sl=sl: e.max(out=vals.ap[:, sl], in_=AT.ap), reads=[AT], writes=[vals])
                    S.op("dve", lambda e, sl=sl: e.max_index(out=idxu.ap[:, sl], in_max=vals.ap[:, sl], in_values=AT.ap), reads=[AT, vals], writes=[idxu])
                    S.op("dve", lambda e, sl=sl: e.match_replace(out=AT.ap, in_to_replace=vals.ap[:, sl], in_values=AT.ap, imm_value=-1.0),
                         reads=[AT, vals], writes=[AT])
                pci = sbt(scB, "pci", [64, 1], I32)
                pcf = sbt(scB, "pcf", [64, 1], F32)
                offs = sbt(scB, "offs", [64, 1], F32)
                tmpo = sbt(scB, "tmpo", [64, 1], F32)
                S.op("pool", lambda e: e.iota(pci.ap, pattern=[[0, 1]], base=0, channel_multiplier=1), writes=[pci])
                CP("dve", pcf.v(), pci.v())
                TS("dve", offs.v(), pcf.v(), 16.0, ALU.is_ge)
                for thr in (32.0, 48.0):
                    TS("dve", tmpo.v(), pcf.v(), thr, ALU.is_ge)
                    TT("dve", offs.v(), offs.v(), tmpo.v(), ALU.add)
                TS("dve", offs.v(), offs.v(), float(L), ALU.mult)
                CP("dve", idxf.v(), idxu.v())
                TS("dve", idxf.v(), idxf.v(), offs.v(), ALU.add)
                TS("dve", idxf.v(), idxf.v(), 0.0, ALU.max, float(NS * L - 1), ALU.min)
                for half in range(2):
                    pa = PA.next()
                    TR(pa[:, 0:64], idxf[:, half * 128:(half + 1) * 128], identf[0:64, 0:64], inc=True)
                    CP("dve", IDXT[:, half, :], pa[:, 0:64])
                    pa2 = PA.next()
                    TR(pa2[:, 0:64], vals[:, half * 128:(half + 1) * 128], identf[0:64, 0:64], inc=True)
                    CP("dve", GT[:, half, :], pa2[:, 0:64])
                S.barrier()

            if debug:
                DMA(DBG_IDX.v(), IDXT.v())
                DMA(DBG_GT.v(), GT.v())
                DMA(DBG_AFF.v(), AFF.v())
            xe = rot(sc, "xe", [128, 1024], BF16, 8)
            xeT = sbt(sc, "xeT", [128, 8, 1024], BF16)
            xeTg = [T(xeT.ap[:, :, g * 128:(g + 1) * 128]) for g in range(8)]
            heT = sbt(sc, "heT", [128, 8, 1024], BF16)
            heTs = [[T(heT.ap[:, fc, sg * 512:(sg + 1) * 512]) for sg in range(2)] for fc in range(8)]
            sgl = rot(sc, "sgl", [128, 512], F32, 2)
            ye = rot(sc, "ye", [128, 1024], F32, 2)
            for g in range(8):
                xg = gather(0, g)
                transposeN(xeTg[g].v(), xg.v(), 8)
            for e_ in range(NE):
                buf = e_ % 2
                pieces = weight_pieces(e_ + 1) if e_ + 1 < NE else []
                nxt = []
                for fc in range(8):
                    for sg in range(2):
                        pgu = PA.next()
                        for m in (0, 1):
                            for kc in range(8):
                                S.op("pe", lambda e, m=m, kc=kc, fc=fc, sg=sg, pgu=pgu: e.matmul(
                                    pgu.ap[:, m * 512:(m + 1) * 512], lhsT=wbf[m][buf].ap[:, kc, fc * 128:(fc + 1) * 128],
                                    rhs=xeT.ap[:, kc, sg * 512:(sg + 1) * 512], start=(kc == 0), stop=(kc == 7)),
                                    reads=[wbfp[m][buf][kc // 2]] + xeTg[sg * 4:(sg + 1) * 4], writes=[pgu], inc=(kc == 7))
                        if pieces:
                            pieces.pop(0)()
                        s1 = sgl.next()
                        ACT(s1.v(), pgu[:, 0:512], AF.Silu)
                        TT("dve", heTs[fc][sg].v(), pgu[:, 512:1024], s1.v(), ALU.mult)
                        if e_ + 1 < NE and fc >= 4:
                            nxt.append(gather(e_ + 1, len(nxt)))
                while pieces:
                    pieces.pop(0)()
                for g in range(8):
                    b, half = g // 2, g % 2
                    col = b * NE + e_
                    pd = PA.next()
                    for hf in range(2):
                        for fc in range(8):
                            S.op("pe", lambda e, hf=hf, fc=fc, g=g, pd=pd: e.matmul(
                                pd.ap[:, hf * 512:(hf + 1) * 512], lhsT=heT.ap[:, fc, g * 128:(g + 1) * 128],
                                rhs=wbf[2][buf].ap[:, fc, hf * 512:(hf + 1) * 512], start=(fc == 0), stop=(fc == 7)),
                                reads=[wbfp[2][buf][fc // 2], heTs[fc][g // 4]], writes=[pd], inc=(fc == 7))
                    y1 = ye.next()
                    STT(y1.v(), pd.v(), GT[:, half, col:col + 1], G2[b].v(), ALU.mult, ALU.mult)
                    S.dma("pool", lambda e, y1=y1, half=half, col=col, b=b: e.indirect_dma_start(
                        out=ACCALL, out_offset=bass.IndirectOffsetOnAxis(ap=IDXT.ap[:, half, col:col + 1], axis=0),
                        in_=y1.ap, in_offset=None, compute_op=ALU.add),
                        reads=[y1, IDXT], writes=[ACC[b]])
                    if e_ + 1 < NE:
                        transposeN(xeTg[g].v(), nxt[g].v(), 8)
            S.barrier()

        with contextlib.ExitStack() as sc:
            L2G = sbt(sc, "L2G", [128, 1024], F32)
            L2B = sbt(sc, "L2B", [128, 1024], F32)
            DMA(L2G.v(), V(ln2_g_d, ln2_g_d.ap.partition_broadcast(128)))
            DMA(L2B.v(), V(ln2_b_d, ln2_b_d.ap.partition_broadcast(128)))
            ain = rot(sc, "ain", [128, 1024], F32, 8)
            aout = rot(sc, "aout", [128, 1024], F32, 8)
            def d_l(i, c):
                b, n = i // NCH, i % NCH
                a = ain.next()
                DMA(a.v(), ACC[b][n * 128:(n + 1) * 128, :])
                c["a"] = a

            def d_l2(i, c):
                pass

            def d_a(i, c):
                c["rs"] = ln_stats(c["a"].v(), 1024, eps=EPS / (ALPHA * ALPHA))

            def d_b(i, c):
                o = aout.next()
                ln_apply(o.v(), c["a"].v(), c["rs"])
                TT("dve", o.v(), o.v(), L2G.v(), ALU.mult)
                TT("pool", o.v(), o.v(), L2B.v(), ALU.add)
                c["o"] = o

            def d_c(i, c):
                b, n = i // NCH, i % NCH
                ot = T(out_d.ap[b * L + n * 128:b * L + (n + 1) * 128, :])
                DMA(ot.v(), c["o"].v())

            pipeline(NS * NCH, [(0, d_l), (4, d_a), (5, d_b), (6, d_c)])
            S.barrier()
        print("kernel: n_ins", S.n_ins, {k: v for k, v in S.cnt.items() if not k.startswith("d")})
    return nc


_NC_CACHE = {}


def kernel(x, c, ctx, c_ctx, w_ada, b_ada, w_in, conv_w, conv_b, conv_ln_g, conv_ln_b, w_conv_out,
           log_decay_f, log_decay_b, ret_gn_g, w_ret_out, w_out, ln1_g, ln1_b,
           w_router, w_gate, w_up, w_down, ln2_g, ln2_b):
    f = lambda a: np.ascontiguousarray(np.asarray(a, dtype=np.float32))
    x, c, ctx, c_ctx = f(x), f(c), f(ctx), f(c_ctx)
    n_cores = 8
    if "nc" not in _NC_CACHE:
        _NC_CACHE["nc"] = build_program()
    nc = _NC_CACHE["nc"]
    convw = np.concatenate([f(conv_w)[0], f(conv_b)[0][None, :]], axis=0)
    shared = {
        "w_ada": f(w_ada)[0], "b_ada": f(b_ada), "w_in": f(w_in)[0], "convw": f(convw),
        "conv_ln_g": f(conv_ln_g), "conv_ln_b": f(conv_ln_b), "w_conv_out": f(w_conv_out)[0],
        "log_decay_f": f(log_decay_f), "log_decay_b": f(log_decay_b), "ret_gn_g": f(ret_gn_g),
        "w_ret_out": f(w_ret_out)[0], "w_out": f(w_out)[0], "ln1_g": f(ln1_g), "ln1_b": f(ln1_b),
        "w_router": f(w_router)[0], "w_gate": f(w_gate)[0], "w_up": f(w_up)[0], "w_down": f(w_down)[0],
        "ln2_g": f(ln2_g), "ln2_b": f(ln2_b),
    }
    in_maps = []
    for i in range(n_cores):
        sl = slice(i * NS, (i + 1) * NS)
        cc = np.concatenate([c[sl], c_ctx[None, :]], axis=0)
        cT = np.ascontiguousarray(cc.T.reshape(8, 128, 5).transpose(1, 0, 2))
        m = dict(shared)
        m["x"] = np.ascontiguousarray(x[sl])
        m["ctx"] = np.ascontiguousarray(ctx[sl])
        m["cT"] = cT
        in_maps.append(m)
    res = run_bass_kernel_spmd(nc, in_maps, core_ids=list(range(n_cores)))
    outs = [np.asarray(r["out"]).reshape(NS, L, D) for r in res.results]
    return np.concatenate(outs, axis=0).astype(np.float32)
```
